# Optimizing a Trainium2 kernel written in Bass

```python
import jax, jax.numpy as jnp
from jax import lax
import numpy as np

D_MODEL = 1024
BATCH = 4
SEQ = 4096
DEPTH = 2

CHUNK = 64
EPS = 1e-6

HEAD_DIM = 64
ATTN_WIDTH = D_MODEL // 2
ATTN_HEADS = ATTN_WIDTH // HEAD_DIM
ROPE_DIM = HEAD_DIM // 4
ROPE_THETA = 500000.0
IDX_HEADS = 4
IDX_DIM = 64
IDX_SCALE = (IDX_DIM ** -0.5) * (IDX_HEADS ** -0.5)
TOPK_MAX = 256
QBLOCK = 64

CONV_WIDTH = D_MODEL // 2
CONV_GROUPS = 8
CONV_K = 3

N_BRANCH = 2
MLP_HIDDEN = 4 * D_MODEL

PROJ_SIZES = (ATTN_WIDTH, ATTN_WIDTH, ATTN_WIDTH, IDX_HEADS * IDX_DIM, IDX_DIM, IDX_HEADS,
              CONV_WIDTH, CONV_WIDTH, CONV_WIDTH, N_BRANCH * D_MODEL)
IN_COLS = sum(PROJ_SIZES)

kernel_name = "hybrid_dsa_shortconv_gated_trunk"


def rmsnorm(x, g):
    xf = x.astype(jnp.float32)
    y = xf * lax.rsqrt(jnp.mean(xf * xf, axis=-1, keepdims=True) + EPS)
    return (y * g.astype(jnp.float32)).astype(x.dtype)


def rope_tables(T):
    inv = 1.0 / (ROPE_THETA ** (jnp.arange(0, ROPE_DIM, 2, dtype=jnp.float32) / ROPE_DIM))
    ang = jnp.arange(T, dtype=jnp.float32)[:, None] * inv[None, :]
    return jnp.cos(ang), jnp.sin(ang)


def apply_partial_rope(x, cos, sin):
    half = ROPE_DIM // 2
    xr = x[..., :ROPE_DIM].astype(jnp.float32)
    x1, x2 = xr[..., :half], xr[..., half:]
    c = cos[None, :, None, :]
    s = sin[None, :, None, :]
    rot = jnp.concatenate([x1 * c - x2 * s, x2 * c + x1 * s], axis=-1)
    return jnp.concatenate([rot.astype(x.dtype), x[..., ROPE_DIM:]], axis=-1)


def dsa_attention(q, k, v, iq, ik, iw):
    B, T, H, Dh = q.shape
    topk = min(TOPK_MAX, T // 4)
    nblk = T // QBLOCK
    key_pos = jnp.arange(T)
    scale = Dh ** -0.5
    ikf = ik.astype(jnp.float32)

    def block(i):
        t0 = i * QBLOCK
        qb = lax.dynamic_slice_in_dim(q, t0, QBLOCK, axis=1)
        iqb = lax.dynamic_slice_in_dim(iq, t0, QBLOCK, axis=1).astype(jnp.float32)
        iwb = lax.dynamic_slice_in_dim(iw, t0, QBLOCK, axis=1).astype(jnp.float32)
        tpos = t0 + jnp.arange(QBLOCK)
        limit = (tpos // CHUNK + 1) * CHUNK
        admissible = key_pos[None, :] < limit[:, None]
        logits = jnp.einsum('bthd,bsd->bths', iqb, ikf)
        score = jnp.einsum('bths,bth->bts', jax.nn.relu(logits), iwb) * IDX_SCALE
        score = jnp.where(admissible[None], score, -jnp.inf)
        _, idx = lax.top_k(score, topk)
        valid = idx < limit[None, :, None]
        kg = jax.vmap(lambda kk, ii: kk[ii])(k, idx)
        vg = jax.vmap(lambda vv, ii: vv[ii])(v, idx)
        s = jnp.einsum('bthd,btkhd->bthk', qb, kg).astype(jnp.float32) * scale
        s = jnp.where(valid[:, :, None, :], s, -jnp.inf)
        p = jax.nn.softmax(s, axis=-1).astype(v.dtype)
        return jnp.einsum('bthk,btkhd->bthd', p, vg)

    out = lax.map(block, jnp.arange(nblk))
    return out.transpose(1, 0, 2, 3, 4).reshape(B, T, H * Dh)


def short_conv(xc, conv_w):
    T = xc.shape[1]
    xp = jnp.pad(xc, ((0, 0), (CONV_K - 1, 0), (0, 0)))
    out = xp[:, 0:T] * conv_w[0]
    for j in range(1, CONV_K):
        out = out + xp[:, j:j + T] * conv_w[j]
    return out


def setup_inputs(seed: int = 0) -> dict:
    key = jax.random.key(seed)
    ks = jax.random.split(key, 12)
    f32 = jnp.float32
    x = jax.random.normal(ks[0], (BATCH, SEQ, D_MODEL), f32)
    norm_mix = 1.0 + 0.02 * jax.random.normal(ks[1], (DEPTH, D_MODEL), f32)
    w_in = jax.random.normal(ks[2], (DEPTH, D_MODEL, IN_COLS), f32) * D_MODEL ** -0.5
    conv_w = jax.random.normal(ks[3], (DEPTH, CONV_K, CONV_WIDTH), f32) * CONV_K ** -0.5
    w_attn_out = jax.random.normal(ks[4], (DEPTH, ATTN_WIDTH, D_MODEL), f32) * ATTN_WIDTH ** -0.5
    w_conv_out = jax.random.normal(ks[5], (DEPTH, CONV_WIDTH, D_MODEL), f32) * CONV_WIDTH ** -0.5
    w_mix_out = jax.random.normal(ks[6], (DEPTH, D_MODEL, D_MODEL), f32) * D_MODEL ** -0.5
    norm_mlp = 1.0 + 0.02 * jax.random.normal(ks[7], (DEPTH, D_MODEL), f32)
    w_mlp_up = jax.random.normal(ks[8], (DEPTH, D_MODEL, MLP_HIDDEN), f32) * D_MODEL ** -0.5
    w_mlp_down = jax.random.normal(ks[9], (DEPTH, MLP_HIDDEN, D_MODEL), f32) * MLP_HIDDEN ** -0.5
    norm_final = 1.0 + 0.02 * jax.random.normal(ks[10], (D_MODEL,), f32)
    return {"x": x, "norm_mix": norm_mix, "w_in": w_in, "conv_w": conv_w,
            "w_attn_out": w_attn_out, "w_conv_out": w_conv_out, "w_mix_out": w_mix_out,
            "norm_mlp": norm_mlp, "w_mlp_up": w_mlp_up, "w_mlp_down": w_mlp_down,
            "norm_final": norm_final}


def reference(x, norm_mix, w_in, conv_w, w_attn_out, w_conv_out, w_mix_out,
              norm_mlp, w_mlp_up, w_mlp_down, norm_final):
    B, T, D = x.shape
    cos, sin = rope_tables(T)
    offsets = np.cumsum(PROJ_SIZES)[:-1].tolist()
    for l in range(DEPTH):
        u = rmsnorm(x, norm_mix[l])
        proj = u @ w_in[l]
        q, k, v, iq, ik, iw, cB, cC, ch, g = jnp.split(proj, offsets, axis=-1)
        q = apply_partial_rope(q.reshape(B, T, ATTN_HEADS, HEAD_DIM), cos, sin)
        k = apply_partial_rope(k.reshape(B, T, ATTN_HEADS, HEAD_DIM), cos, sin)
        v = v.reshape(B, T, ATTN_HEADS, HEAD_DIM)
        iq = apply_partial_rope(iq.reshape(B, T, IDX_HEADS, IDX_DIM), cos, sin)
        ik = apply_partial_rope(ik[:, :, None, :], cos, sin)[:, :, 0, :]
        y_attn = dsa_attention(q, k, v, iq, ik, iw) @ w_attn_out[l]
        y_conv = (cB * short_conv(cC * ch, conv_w[l])) @ w_conv_out[l]
        gates = jax.nn.sigmoid(g.reshape(B, T, N_BRANCH, D))
        merged = gates[:, :, 0] * y_attn + gates[:, :, 1] * y_conv
        x = x + merged @ w_mix_out[l]
        u = rmsnorm(x, norm_mlp[l])
        x = x + jnp.square(jax.nn.relu(u @ w_mlp_up[l])) @ w_mlp_down[l]
    return rmsnorm(x, norm_final)
```

```python
import os
import numpy as np
from contextlib import ExitStack
import concourse.bass as bass
import concourse.mybir as mybir
from concourse.bass_utils import run_bass_kernel_spmd

F32 = mybir.dt.float32
BF16 = mybir.dt.bfloat16
AF = mybir.ActivationFunctionType
ALU = mybir.AluOpType

D = 1024
T = 4096
NB = 4
DEPTH = 2
NCORES = 8
TL = 2048
NT = 16
INC = 5444
C_Q, C_K, C_V, C_IQ, C_IK, C_IW, C_CB, C_CC, C_CH, C_G = 0, 512, 1024, 1536, 1792, 1856, 1860, 2372, 2884, 3396
HID = 4096
EPS = 1e-6
XS_ROWS = 1096
NEG = -30000.0
BIS_ITERS = 10
BIS_R = 4.0
ACT_BISECT = False
SAME_ENG_SYNC = True
NDSEM = 32
NCONV = 48
CUT = int(os.environ.get('K_CUT', '99'))


def OP(name, *a, **k):
    return (name, a, k)


class Buf:
    __slots__ = ("name", "w", "r")

    def __init__(self, name):
        self.name = name
        self.w = []
        self.r = {}


class Sched:
    def __init__(self, nc, sems, dsems):
        self.nc = nc
        self.q = {k: [] for k in ("pe", "act", "dve", "pool", "sp")}
        self.cnt = {k: 0 for k in self.q}
        self.cnt["cc"] = 0
        self.known = {k: {} for k in self.q}
        self.sems = sems
        self.dsems = dsems
        self.dcnt = [0] * len(dsems)
        self.dnext = 0
        self.mute = False

    def _wait(self, eng, tok):
        kind, key, val = tok
        if kind == "e" and key == eng and (eng == "pe" or not SAME_ENG_SYNC):
            return
        k = (kind, key)
        if self.known[eng].get(k, 0) >= val:
            return
        self.known[eng][k] = val
        sem = self.sems[key] if kind == "e" else self.dsems[key]
        self.q[eng].append(OP("wait_ge", sem, val))

    def _deps(self, eng, reads, writes):
        for b in reads:
            for t in b.w:
                self._wait(eng, t)
        for b in writes:
            for t in b.w:
                self._wait(eng, t)
            for t in b.r.values():
                self._wait(eng, t)

    def _mark(self, tok, reads, writes, append=False):
        for b in reads:
            b.r[(tok[0], tok[1])] = tok
        for b in writes:
            if append:
                b.w.append(tok)
            else:
                b.w = [tok]
                b.r = {}

    def op(self, eng, fn, reads=(), writes=(), sig=True):
        if self.mute:
            return
        self._deps(eng, reads, writes)
        if sig:
            self.cnt[eng] += 1
            tok = ("e", eng, self.cnt[eng])
            sem = self.sems[eng]
            self.q[eng].append(lambda e, fn=fn, sem=sem: getattr(e, fn[0])(*fn[1], **fn[2]).then_inc(sem, 1))
        else:
            tok = ("e", eng, self.cnt[eng] + 1)
            self.q[eng].append(lambda e, fn=fn: getattr(e, fn[0])(*fn[1], **fn[2]))
        self._mark(tok, reads, writes)

    def dma(self, qeng, out_ap, in_ap, reads=(), writes=(), append=False, fixed=None, **kw):
        if self.mute:
            return
        self._deps(qeng, reads, writes)
        if fixed is not None:
            i = NDSEM + fixed
        else:
            i = self.dnext
            self.dnext = (i + 1) % NDSEM
        if self.dcnt[i] > 0:
            self._wait(qeng, ("d", i, 16 * self.dcnt[i]))
        self.dcnt[i] += 1
        tok = ("d", i, 16 * self.dcnt[i])
        sem = self.dsems[i]
        self.q[qeng].append(
            lambda e, o=out_ap, a=in_ap, sem=sem, kw=kw: e.dma_start(out=o, in_=a, **kw).then_inc(sem, 16))
        self._mark(tok, reads, writes, append=append)

    def collective(self, ins_ap, outs_ap, reads, writes):
        if self.mute:
            return
        eng = "pool"
        self._deps(eng, reads, writes)
        self.cnt["cc"] += 1
        tok = ("e", "cc", self.cnt["cc"])
        sem = self.sems["cc"]
        groups = [[0, 1], [2, 3], [4, 5], [6, 7]]
        self.q[eng].append(
            lambda e, a=ins_ap, o=outs_ap, sem=sem: e.collective_compute(
                "AllGather", ALU.bypass, replica_groups=groups, ins=[a], outs=[o]).then_inc(sem))
        self._mark(tok, reads, writes)

    def barrier(self):
        toks = [("e", k, self.cnt[k]) for k in ("pe", "act", "dve", "pool", "sp", "cc") if self.cnt[k] > 0]
        toks += [("d", i, 16 * c) for i, c in enumerate(self.dcnt) if c > 0]
        for eng in self.q:
            for t in toks:
                if t[0] == "e" and t[1] == eng:
                    continue
                self._wait(eng, t)

    def flush(self, block):
        q = self.q

        def run(lst, e):
            for f in lst:
                if isinstance(f, tuple):
                    getattr(e, f[0])(*f[1], **f[2])
                else:
                    f(e)

        @block.tensor
        def _(e):
            run(q["pe"], e)

        @block.scalar
        def _(e):
            run(q["act"], e)

        @block.vector
        def _(e):
            run(q["dve"], e)

        @block.gpsimd
        def _(e):
            run(q["pool"], e)

        @block.sync
        def _(e):
            run(q["sp"], e)

        self.q = {k: [] for k in q}


def build_program(debug=False, nlayers=DEPTH, stop_after=None):
    nc = bass.Bass("TRN2", target_bir_lowering=False)
    dt = nc.dram_tensor
    x_in = dt("x", [TL, D], F32, kind="ExternalInput")
    w_in = dt("w_in", [DEPTH, D, INC], F32, kind="ExternalInput")
    w_ao = dt("w_attn_out", [DEPTH, 512, D], F32, kind="ExternalInput")
    w_co = dt("w_conv_out", [DEPTH, 512, D], F32, kind="ExternalInput")
    w_mx = dt("w_mix_out", [DEPTH, D, D], F32, kind="ExternalInput")
    w_up = dt("w_mlp_up", [DEPTH, D, HID], F32, kind="ExternalInput")
    w_dn = dt("w_mlp_down", [DEPTH, HID, D], F32, kind="ExternalInput")
    n_mix = dt("norm_mix", [DEPTH, D], F32, kind="ExternalInput")
    n_mlp = dt("norm_mlp", [DEPTH, D], F32, kind="ExternalInput")
    n_fin = dt("norm_final", [D], F32, kind="ExternalInput")
    conv_w = dt("conv_w", [DEPTH, 3, 512], F32, kind="ExternalInput")
    ropeC = dt("ropeC", [16, TL], F32, kind="ExternalInput")
    ropeS = dt("ropeS", [16, TL], F32, kind="ExternalInput")
    adm_in = dt("adm", [128, 2, 512], F32, kind="ExternalInput")
    sel_in = dt("sel", [128, 2], F32, kind="ExternalInput")
    ident_in = dt("ident", [128, 128], F32, kind="ExternalInput")
    out_d = dt("out", [TL, D], F32, kind="ExternalOutput")
    dbg = {}
    if debug:
        for nm, shp, ty in [("d_ut", [D, TL], BF16), ("d_xs", [XS_ROWS, TL], BF16), ("d_qt", [512, TL], BF16),
                            ("d_iqt", [256, TL], BF16), ("d_at", [512, TL], BF16), ("d_aw", [128, 64], F32),
                            ("d_sg", [128, 64], F32), ("d_sc", [128, 4096], F32), ("d_mb", [128, 8192], BF16),
                            ("d_cb", [128, 1], F32)]:
            dbg[nm] = dt(nm, shp, ty, kind="ExternalOutput")

    WinB = [dt(f"WinB{l}", [D, INC], BF16) for l in range(DEPTH)]
    WaoB = [dt(f"WaoB{l}", [512, D], BF16) for l in range(DEPTH)]
    WcoB = [dt(f"WcoB{l}", [512, D], BF16) for l in range(DEPTH)]
    WmxB = [dt(f"WmxB{l}", [D, D], BF16) for l in range(DEPTH)]
    WupB = [dt(f"WupB{l}", [D, HID], BF16) for l in range(DEPTH)]
    WdnB = [dt(f"WdnB{l}", [HID, D], BF16) for l in range(DEPTH)]
    UTd = dt("UTd", [D, TL], BF16)
    CHd = dt("CHd", [512, NT * 130], BF16)
    XRd = dt("XRd", [TL, D], F32)
    PR = [256, 320, 256, 264]
    XSp = [[dt(f"XSp{l}_{k}", [PR[k], TL], BF16) for k in range(4)] for l in range(DEPTH)]
    XGp = [[dt(f"XGp{l}_{k}", [2 * PR[k], TL], BF16) for k in range(4)] for l in range(DEPTH)]

    b_W = {(nm, l): Buf(f"{nm}{l}") for nm in ("in", "ao", "co", "mx", "up", "dn") for l in range(DEPTH)}
    b_UTd, b_CHd, b_XRd = Buf("UTd"), Buf("CHd"), Buf("XRd")
    b_XSd = [Buf("XSd0"), Buf("XSd1")]
    b_XGd = [Buf("XGd0"), Buf("XGd1")]
    b_out = Buf("out")
    b_dbg = Buf("dbg")

    with ExitStack() as top:
        sems = {k: top.enter_context(nc.semaphore(f"s_{k}")) for k in ("pe", "act", "dve", "pool", "sp", "cc")}
        dsems = [top.enter_context(nc.semaphore(f"d_{i}")) for i in range(NDSEM + NCONV)]
        S = Sched(nc, sems, dsems)

        uid = [0]

        def sb(st, name, shape, dtype):
            uid[0] += 1
            return st.enter_context(nc.sbuf_tensor(f"{name}_{uid[0]}", shape, dtype))

        AT = sb(top, "AT", [128, 4, TL], BF16)
        AW = sb(top, "AW", [128, NT, 4], F32)
        SG = sb(top, "SG", [128, NT, 4], F32)
        IDF = sb(top, "IDF", [128, 128], F32)
        IDB = sb(top, "IDB", [128, 128], BF16)
        ONES = sb(top, "ONES", [128, 128], BF16)
        ADM = sb(top, "ADM", [128, 2, 512], F32)
        SEL = sb(top, "SEL", [128, 2], F32)
        GM = sb(top, "GM", [128, DEPTH, 8], F32)
        GP = sb(top, "GP", [128, DEPTH, 8], F32)
        CW = sb(top, "CW", [128, DEPTH, 4, 3], F32)
        b_QT, b_IQT, b_AT, b_AW, b_SG = Buf("QT"), Buf("IQT"), Buf("AT"), Buf("AW"), Buf("SG")
        b_const = Buf("const")
        PS = [top.enter_context(nc.psum_tensor(f"ps{i}", [128, 512], F32)) for i in range(8)]
        b_PS = [Buf(f"ps{i}") for i in range(8)]

        with nc.Block() as block:
            S.dma("sp", IDF[:, :], ident_in[:, :], writes=[b_const], append=True)
            S.dma("pool", IDB[:, :], ident_in[:, :], writes=[b_const], append=True, fixed=40)
            S.dma("sp", ADM[:, :, :], adm_in[:, :, :], writes=[b_const], append=True)
            S.dma("sp", SEL[:, :], sel_in[:, :], writes=[b_const], append=True)
            for l in range(DEPTH):
                S.dma("sp", GM[:, l, :], n_mix[l, :].rearrange("(k p) -> p k", p=128), writes=[b_const],
                      append=True, allow_slow_non_contiguous=True)
                S.dma("sp", GP[:, l, :], n_mlp[l, :].rearrange("(k p) -> p k", p=128), writes=[b_const],
                      append=True, allow_slow_non_contiguous=True)
                for jt in range(3):
                    S.dma("sp", CW[:, l, :, jt], conv_w[l, jt, :].rearrange("(c p) -> p c", p=128),
                          writes=[b_const], append=True, allow_slow_non_contiguous=True)
            S.op("pool", OP("memset", ONES[:, :], 1.0), writes=[b_const])
            with ExitStack() as st0:
                stg = [sb(st0, f"stg{i}", [128, 2048], F32) for i in range(4)]
                stb = [sb(st0, f"stb{i}", [128, 2048], BF16) for i in range(4)]
                b_stg = [Buf(f"stg{i}") for i in range(4)]
                b_stb = [Buf(f"stb{i}") for i in range(4)]
                tiles_ = [(r0, c0, n_) for r0 in range(0, D, 128) for (c0, n_) in ((0, 2048), (2048, 2048), (4096, 1348))]

                def ld(i):
                    r0, c0, n_ = tiles_[i]
                    S.dma("sp", stg[i % 4][:, 0:n_], w_in[0, r0:r0 + 128, c0:c0 + n_], writes=[b_stg[i % 4]])
                for i in range(4):
                    ld(i)
                for i, (r0, c0, n_) in enumerate(tiles_):
                    k = i % 4
                    if i % 2 == 0:
                        S.op("dve", OP("tensor_copy", stb[k][:, 0:n_], stg[k][:, 0:n_]), reads=[b_stg[k]],
                             writes=[b_stb[k]])
                    else:
                        S.op("act", OP("activation", stb[k][:, 0:n_], stg[k][:, 0:n_], AF.Copy), reads=[b_stg[k]],
                             writes=[b_stb[k]])
                    S.dma("sp", WinB[0][r0:r0 + 128, c0:c0 + n_], stb[k][:, 0:n_], reads=[b_stb[k]],
                          writes=[b_W[("in", 0)]], append=True)
                    if i + 4 < len(tiles_):
                        ld(i + 4)
            S.flush(block)

        if stop_after == "init":
            return nc
        def conversion_thunks():
            th = []
            ncv = 0
            for l_ in range(nlayers):
                for (nm, src, dst, rows, rb) in (("in", w_in, WinB, D, 256), ("ao", w_ao, WaoB, 512, 512),
                                                 ("co", w_co, WcoB, 512, 512), ("mx", w_mx, WmxB, D, 512),
                                                 ("up", w_up, WupB, D, 256), ("dn", w_dn, WdnB, HID, 512)):
                    if l_ == 0 and nm == "in":
                        continue
                    for r0 in range(0, rows, rb):
                        def cv(l_=l_, nm=nm, src=src, dst=dst, r0=r0, rb=rb, ncv=ncv):
                            S.dma("pool",
                                  dst[l_][r0:r0 + rb, :].rearrange("r c -> (r c)").rearrange("(a b) -> a b", a=16),
                                  src[l_, r0:r0 + rb, :].rearrange("r c -> (r c)").rearrange("(a b) -> a b", a=16),
                                  writes=[b_W[(nm, l_)]], append=True, fixed=ncv)
                        th.append(cv)
                        ncv += 1
            return th

        conv_th = conversion_thunks()

        def norm_group(bufs, xt, b_xt, gvec_ap, dst, b_dst):
            XS2, SSQ, b_x2, b_ss = bufs
            for tau in range(4):
                S.op("act", OP("activation", XS2[tau % 2][:, :], xt[:, tau, :], AF.Square,
                               accum_out=SSQ[:, tau:tau + 1]), reads=[b_xt], writes=[b_x2[tau % 2], b_ss])
            S.op("dve", OP("tensor_scalar", SSQ[:, 4:8], SSQ[:, 0:4], 1.0 / D, EPS, ALU.mult, ALU.add),
                 reads=[b_ss], writes=[b_ss])
            S.op("act", OP("activation", SSQ[:, 8:12], SSQ[:, 4:8], AF.Sqrt), reads=[b_ss], writes=[b_ss])
            S.op("dve", OP("reciprocal", SSQ[:, 12:16], SSQ[:, 8:12]), reads=[b_ss], writes=[b_ss])
            for tau in range(4):
                x2, bx = XS2[tau % 2], b_x2[tau % 2]
                c0 = tau * 128
                S.op("dve", OP("tensor_scalar", x2[:, :], xt[:, tau, :], SSQ[:, 12 + tau:13 + tau], None, ALU.mult),
                     reads=[b_xt, b_ss], writes=[bx])
                for half in range(2):
                    bk = 6 + half
                    for k4 in range(4):
                        kc = half * 4 + k4
                        S.op("pe", OP("transpose", PS[bk][:, k4 * 128:(k4 + 1) * 128],
                                      x2[:, kc * 128:(kc + 1) * 128], IDF[:, :]),
                             reads=[bx, b_const], writes=[b_PS[bk]], sig=(k4 == 3))
                    for k4 in range(4):
                        kc = half * 4 + k4
                        S.op("act", OP("activation", dst[:, kc, c0:c0 + 128], PS[bk][:, k4 * 128:(k4 + 1) * 128],
                                       AF.Copy, scale=gvec_ap[:, kc:kc + 1]),
                             reads=[b_PS[bk], b_const], writes=[b_dst])

        class WStream:
            def __init__(self, st, nbuf, loads, live):
                self.ahead = nbuf - live
                self.bufs = [sb(st, f"WB{i}", [128, 4096], BF16) for i in range(nbuf)]
                self.bb = [Buf(f"WB{i}") for i in range(nbuf)]
                self.loads = loads
                self.issued = 0
                self.nbuf = nbuf

            def _issue(self):
                i = self.issued
                src, k, c, wbuf = self.loads[i]
                t = self.bufs[i % self.nbuf]
                dst = t[:, 0:k * c].rearrange("p (k c) -> p k c", k=k)
                S.dma("sp", dst, src, reads=[wbuf], writes=[self.bb[i % self.nbuf]])
                self.issued += 1

            def get(self, i):
                while self.issued < min(len(self.loads), i + self.ahead + 1):
                    self._issue()
                src, k, c, wbuf = self.loads[i]
                t = self.bufs[i % self.nbuf]
                return t[:, 0:k * c].rearrange("p (k c) -> p k c", k=k), self.bb[i % self.nbuf]

        def wsrc(tensor_l, c0, ncols, k0=0, nk=8):
            return tensor_l.ap().rearrange("(k p) c -> p k c", p=128)[:, k0:k0 + nk, c0:c0 + ncols]

        for l in range(nlayers):
            x_src, b_xsrc = (x_in, b_const) if l == 0 else (XRd, b_XRd)
            last = (l == nlayers - 1)
            ab = ExitStack()
            QT = sb(ab, "QT", [128, 4, TL], BF16)
            IQT = sb(ab, "IQT", [128, 2, TL], BF16)
            with ExitStack() as st, nc.Block() as block:
                CT = sb(st, "CT", [128, TL], F32)
                STb = sb(st, "STb", [128, TL], F32)
                XT = [sb(st, f"XTa{i}", [128, 4, D], F32) for i in range(2)]
                UTg = [sb(st, f"UTg{i}", [128, 8, 512], BF16) for i in range(2)]
                WSW = sb(st, "WSW", [128, 11, 8, 128], BF16)
                KS = sb(st, "KS", [128, 4, 512], BF16)
                IKS = sb(st, "IKS", [64, 512], BF16)
                VS = sb(st, "VS", [128, 4, 512], BF16)
                CS = [sb(st, f"CS{i}", [128, 512], F32) for i in range(2)]
                CHS = sb(st, "CHS", [128, 4, 4, 128], BF16)
                HS = sb(st, "HS", [128, 4, NT, 2], BF16)
                T1 = [sb(st, f"T1{i}", [128, 512], F32) for i in range(2)]
                T2 = [sb(st, f"T2{i}", [128, 512], F32) for i in range(2)]
                XS2 = [sb(st, f"XS2a{i}", [128, D], F32) for i in range(2)]
                SSQ = sb(st, "SSQ", [128, 16], F32)
                b_rope, b_XT, b_UTg, b_WSW = Buf("rope"), [Buf("XT0"), Buf("XT1")], [Buf("UTg0"), Buf("UTg1")], \
                    Buf("WSW")
                b_KS, b_IKS, b_VS, b_CS, b_CHS, b_HS = Buf("KS"), Buf("IKS"), Buf("VS"), [Buf("CS0"), Buf("CS1")], \
                    Buf("CHS"), Buf("HS")
                b_T1, b_T2 = [Buf("T10"), Buf("T11")], [Buf("T20"), Buf("T21")]
                b_x2a, b_ssa = [Buf("x2a0"), Buf("x2a1")], Buf("ssa")
                S.op("dve", OP("memset", CT[:, :], 1.0), writes=[b_rope])
                S.op("dve", OP("memset", STb[:, :], 0.0), writes=[b_rope])
                for r0 in (0, 64):
                    S.dma("sp", CT[r0:r0 + 16, :], ropeC[:, :], writes=[b_rope], append=True)
                    S.dma("sp", STb[r0:r0 + 16, :], ropeS[:, :], writes=[b_rope], append=True)
                loads = [(wsrc(WinB[l], C_Q, 512), 8, 512, b_W[("in", l)]),
                         (wsrc(WinB[l], C_K, 512), 8, 512, b_W[("in", l)]),
                         (wsrc(WinB[l], C_IQ, 324), 8, 324, b_W[("in", l)])]
                for G in range(4):
                    loads += [(wsrc(WinB[l], C_Q, 512), 8, 512, b_W[("in", l)]),
                              (wsrc(WinB[l], C_K, 512), 8, 512, b_W[("in", l)]),
                              (wsrc(WinB[l], C_V, 512), 8, 512, b_W[("in", l)]),
                              (wsrc(WinB[l], C_IQ, 324), 8, 324, b_W[("in", l)]),
                              (wsrc(WinB[l], C_CC, 512), 8, 512, b_W[("in", l)]),
                              (wsrc(WinB[l], C_CH, 512), 8, 512, b_W[("in", l)])]
                ws = WStream(st, 4, loads, 2)
                xsp = XSp[l]
                swi = [0]
                bank = [0]

                def fm_proj(Wt, b_w, c0, nf, utg, b_utg, bk):
                    for kc in range(8):
                        S.op("pe", OP("matmul", PS[bk][0:nf, :], Wt[:, kc, c0:c0 + nf], utg[:, kc, :],
                                                             start=(kc == 0), stop=(kc == 7)),
                             reads=[b_w, b_utg], writes=[b_PS[bk]], sig=(kc == 7))

                ci = 0
                for li, nch_, in ((0, 4), (1, 4), (2, 3)):
                    Wt0, b_w0 = ws.get(li)
                    for c in range(nch_):
                        nf = 64 if (li == 2 and c == 2) else 128
                        c0 = c * 128
                        S.op("dve", OP("tensor_copy", WSW[:, ci, :, 0:nf], Wt0[:, :, c0:c0 + nf]), reads=[b_w0],
                             writes=[b_WSW])
                        for hb in range(0, nf, 64):
                            S.op("dve", OP("tensor_copy", WSW[:, ci, :, hb:hb + 8], Wt0[:, :, c0 + hb + 8:c0 + hb + 16]),
                                 reads=[b_w0], writes=[b_WSW])
                            S.op("dve", OP("tensor_copy", WSW[:, ci, :, hb + 8:hb + 16], Wt0[:, :, c0 + hb:c0 + hb + 8]),
                                 reads=[b_w0], writes=[b_WSW])
                        ci += 1

                def rope_chunk(Wt, b_w, c0, nf, utg, b_utg, dst_ap, b_dst, g0, ci):
                    si = swi[0] % 2
                    swi[0] += 1
                    bq = bank[0] % 6
                    bs = (bank[0] + 1) % 6
                    bank[0] += 2
                    fm_proj(Wt, b_w, c0, nf, utg, b_utg, bq)
                    fm_proj(WSW[:, ci, :, :], b_WSW, 0, nf, utg, b_utg, bs)
                    S.op("dve", OP("tensor_tensor", T1[si][0:nf, :], PS[bs][0:nf, :], STb[0:nf, g0:g0 + 512], ALU.mult),
                         reads=[b_PS[bs], b_rope], writes=[b_T1[si]])
                    S.op("dve", OP("tensor_tensor", T2[si][0:nf, :], PS[bq][0:nf, :], CT[0:nf, g0:g0 + 512], ALU.mult),
                         reads=[b_PS[bq], b_rope], writes=[b_T2[si]])
                    S.op("dve", OP("tensor_tensor", dst_ap, T1[si][0:nf, :], T2[si][0:nf, :], ALU.add),
                         reads=[b_T1[si], b_T2[si]], writes=[b_dst])

                def xload(G_):
                    S.dma("sp", XT[G_ % 2][:, :, :],
                          x_src[G_ * 512:G_ * 512 + 512, :].rearrange("(t p) d -> p t d", p=128),
                          reads=[b_xsrc], writes=[b_XT[G_ % 2]])

                def head_norm(G_):
                    k_ = G_ % 2
                    norm_group((XS2, SSQ, b_x2a, b_ssa), XT[k_], b_XT[k_], GM[:, l, :], UTg[k_], b_UTg[k_])
                    S.dma("sp", UTd.ap().rearrange("(k p) t -> p k t", p=128)[:, :, G_ * 512:G_ * 512 + 512],
                          UTg[k_][:, :, :], reads=[b_UTg[k_]], writes=[b_UTd], append=True)

                xload(0)
                head_norm(0)
                for G in range(4):
                    g0 = G * 512
                    xb = G % 2
                    xt, utg = XT[xb], UTg[xb]
                    if G < 3:
                        xload(G + 1)
                    S.mute = CUT < 2
                    Wt, b_w = ws.get(3 + G * 6 + 0)
                    for c in range(4):
                        rope_chunk(Wt, b_w, c * 128, 128, utg, b_UTg[xb], QT[:, c, g0:g0 + 512], b_QT, g0, c)
                    S.mute = CUT < 3
                    Wt, b_w = ws.get(3 + G * 6 + 1)
                    for c in range(4):
                        rope_chunk(Wt, b_w, c * 128, 128, utg, b_UTg[xb], KS[:, c, :], b_KS, g0, 4 + c)
                    for hh in range(2):
                        S.dma("sp", xsp[hh][0:256, :].rearrange("(c p) t -> p c t", p=128)[:, :, g0:g0 + 512],
                              KS[:, 2 * hh:2 * hh + 2, :], reads=[b_KS], writes=[b_XSd[l]], append=True)
                    S.mute = CUT < 4
                    Wt, b_w = ws.get(3 + G * 6 + 2)
                    for tau in range(4):
                        bk = bank[0] % 6
                        bank[0] += 1
                        for kc in range(8):
                            S.op("pe", OP("matmul",
                                PS[bk][:, :], utg[:, kc, tau * 128:(tau + 1) * 128], Wt[:, kc, :],
                                start=(kc == 0), stop=(kc == 7)),
                                 reads=[b_w, b_UTg[xb]], writes=[b_PS[bk]], sig=(kc == 7))
                        S.op("act", OP("activation", VS[:, tau, :], PS[bk][:, :], AF.Copy),
                             reads=[b_PS[bk]], writes=[b_VS])
                    vview = xsp[2 + G // 2][0:256, :].rearrange("r (q c) -> (r q) c", q=4)
                    S.dma("sp", vview[(G % 2) * 512:(G % 2) * 512 + 512, :].rearrange("(t p) c -> p t c", p=128),
                          VS[:, :, :],
                          reads=[b_VS], writes=[b_XSd[l]], append=True)
                    S.mute = CUT < 5
                    Wt, b_w = ws.get(3 + G * 6 + 3)
                    for c in range(2):
                        rope_chunk(Wt, b_w, c * 128, 128, utg, b_UTg[xb], IQT[:, c, g0:g0 + 512], b_IQT, g0, 8 + c)
                    rope_chunk(Wt, b_w, 256, 64, utg, b_UTg[xb], IKS[:, :], b_IKS, g0, 10)
                    S.dma("sp", xsp[1][256:320, g0:g0 + 512], IKS[:, :], reads=[b_IKS], writes=[b_XSd[l]], append=True)
                    for tau in range(4):
                        j = G * 4 + tau
                        bk = bank[0] % 6
                        bank[0] += 1
                        for kc in range(8):
                            S.op("pe", OP("matmul",
                                PS[bk][:, 0:4], utg[:, kc, tau * 128:(tau + 1) * 128], Wt[:, kc, 320:324],
                                start=(kc == 0), stop=(kc == 7)),
                                 reads=[b_w, b_UTg[xb]], writes=[b_PS[bk]], sig=(kc == 7))
                        S.op("act", OP("activation", AW[:, j, :], PS[bk][:, 0:4], AF.Abs, scale=1.0 / 16.0),
                             reads=[b_PS[bk]], writes=[b_AW])
                        S.op("act", OP("activation", SG[:, j, :], PS[bk][:, 0:4], AF.Sign),
                             reads=[b_PS[bk]], writes=[b_SG])
                    if G < 3:
                        head_norm(G + 1)
                    S.mute = CUT < 6
                    WtC, b_wC = ws.get(3 + G * 6 + 4)
                    WtH, b_wH = ws.get(3 + G * 6 + 5)
                    for c in range(4):
                        bc = bank[0] % 6
                        bh = (bank[0] + 1) % 6
                        bank[0] += 2
                        fm_proj(WtC, b_wC, c * 128, 128, utg, b_UTg[xb], bc)
                        fm_proj(WtH, b_wH, c * 128, 128, utg, b_UTg[xb], bh)
                        S.op("act", OP("activation", CS[c % 2][:, :], PS[bc][:, :], AF.Copy),
                             reads=[b_PS[bc]], writes=[b_CS[c % 2]])
                        S.op("dve", OP("tensor_tensor",
                            CHS[:, c, :, :], CS[c % 2][:, :].rearrange("p (t w) -> p t w", t=4),
                            PS[bh][:, :].rearrange("p (t w) -> p t w", t=4), ALU.mult),
                             reads=[b_PS[bh], b_CS[c % 2]], writes=[b_CHS])
                    chv = CHd.ap().rearrange("(c p) (j w) -> p c j w", p=128, w=130)
                    for c in range(4):
                        S.dma("sp", chv[:, c, G * 4:G * 4 + 4, 2:130], CHS[:, c, :, :], reads=[b_CHS], writes=[b_CHd],
                              append=True)
                    S.op("dve", OP("tensor_copy", HS[:, :, G * 4:G * 4 + 4, :], CHS[:, :, :, 126:128]),
                         reads=[b_CHS], writes=[b_HS])
                S.mute = CUT < 7
                hv = xsp[3][256:264, :].rearrange("r (a b) -> (r a) b", b=32).rearrange("(c p) b -> p c b", p=128)
                S.dma("sp", hv, HS[:, :, :, :].rearrange("p c j w -> p c (j w)"), reads=[b_HS], writes=[b_XSd[l]],
                      append=True)
                S.mute = False
                if debug and l == 0:
                    S.dma("sp", dbg["d_ut"][:, :], UTd[:, :], reads=[b_UTd], writes=[b_dbg], append=True)
                    S.dma("sp", dbg["d_qt"].ap().rearrange("(c p) t -> p c t", p=128), QT[:, :, :], reads=[b_QT],
                          writes=[b_dbg], append=True)
                    S.dma("sp", dbg["d_iqt"].ap().rearrange("(c p) t -> p c t", p=128), IQT[:, :, :], reads=[b_IQT],
                          writes=[b_dbg], append=True)
                    S.dma("sp", dbg["d_aw"][:, :], AW[:, :, :].rearrange("p j h -> p (j h)"), reads=[b_AW],
                          writes=[b_dbg], append=True)
                    S.dma("sp", dbg["d_sg"][:, :], SG[:, :, :].rearrange("p j h -> p (j h)"), reads=[b_SG],
                          writes=[b_dbg], append=True)
                S.mute = CUT < 8
                for kk in range(4):
                    S.collective(xsp[kk].ap().opt(), XGp[l][kk].ap().opt(), reads=[b_XSd[l]], writes=[b_XGd[l]])
                S.mute = False
                S.barrier()
                S.flush(block)

            if stop_after == f"A{l}":
                ab.close()
                return nc
            with ExitStack() as st, nc.Block() as block:
                KT = sb(st, "KT", [128, 4, T], BF16)
                VA = sb(st, "VA", [128, 32, 512], BF16)
                IKT = sb(st, "IKT", [128, T], BF16)
                SC1 = sb(st, "SC", [128, T], F32)
                SCs = [SC1, SC1]
                MB = sb(st, "MB", [128, 2, T], BF16)
                MTs = [sb(st, f"MT{i}", [128, 32, 256], BF16) for i in range(2)]
                RR = [[sb(st, f"RR{i}{h}", [128, 512], BF16) for h in range(4)] for i in range(2)]
                PT = [sb(st, f"PT{i}", [128, 512], BF16) for i in range(4)]
                DGs = [sb(st, f"DG{i}", [128, 4, 128], BF16) for i in range(2)]
                CBd = sb(st, "CBd", [128, 8], F32)
                CBa = sb(st, "CBa", [128, 4], F32)
                CBf = sb(st, "CBf", [128, 2], F32)
                RC = [sb(st, f"RC{i}", [128, 512], F32) for i in range(2)]
                QBD = [sb(st, f"QBD{i}", [128, 4, 512], BF16) for i in range(2)]
                b_QBD = [Buf("QBD0"), Buf("QBD1")]
                for i_ in range(2):
                    S.op("pool", OP("memset", QBD[i_][:, :, :], 0.0), writes=[b_QBD[i_]])
                b_KT, b_VA, b_IKT, b_MTs = Buf("KT"), Buf("VA"), Buf("IKT"), [Buf("MT0"), Buf("MT1")]
                b_sc1 = Buf("SC")
                b_SC, b_MB, b_DG = [b_sc1, b_sc1], [Buf("MB0"), Buf("MB1")], [Buf("DG0"), Buf("DG1")]
                b_JKd, b_JKa = b_MB[0], b_MB[1]
                b_CBd2 = [Buf("CBd0"), Buf("CBd1")]
                b_RR = [[Buf(f"RR{i}{h}") for h in range(4)] for i in range(2)]
                b_PT, b_CBd, b_CBa, b_CBf, b_RC = [Buf("PT0"), Buf("PT1"), Buf("PT2"), Buf("PT3")], Buf("CBd"), Buf("CBa"), \
                    [Buf("CBf0"), Buf("CBf1")], [Buf("RC0"), Buf("RC1")]
                xgp = XGp[l]
                for pp in range(2):
                    for hp in range(4):
                        kk = hp // 2
                        r0 = pp * PR[kk] + (hp % 2) * 128
                        S.dma("sp", KT[:, hp, :].rearrange("p (j q i) -> p j q i", q=2, i=128)[:, :, pp, :],
                              xgp[kk][r0:r0 + 128, :].rearrange("p (j i) -> p j i", i=128),
                              reads=[b_XGd[l]], writes=[b_KT], append=True)
                    for r0 in (0, 64):
                        S.dma("sp", IKT[r0:r0 + 64, :].rearrange("p (j q i) -> p j q i", q=2, i=128)[:, :, pp, :],
                              xgp[1][pp * 320 + 256:pp * 320 + 320, :].rearrange("p (j i) -> p j i", i=128),
                              reads=[b_XGd[l]], writes=[b_IKT], append=True)
                for j in range(NT):
                    for pp in range(2):
                        kk = 2 + j // 8
                        vview = xgp[kk][pp * PR[kk]:pp * PR[kk] + 256, :].rearrange("r (q c) -> (r q) c", q=4)
                        jj = j % 8
                        S.dma("sp", VA[:, 2 * j + pp, :], vview[jj * 128:(jj + 1) * 128, :], reads=[b_XGd[l]],
                              writes=[b_VA], append=True)
                lrot = [0]

                def ib_units(g):
                    nch = g + 1
                    N = 512 * nch
                    nkb = 4 * nch
                    MT, b_MT = MTs[g % 2], b_MTs[g % 2]
                    U = []
                    wK = BIS_R / (2.0 ** BIS_ITERS)
                    for tau in range(2):
                        j = 2 * g + tau
                        SC, b_sc, DG, b_dg = SC1, b_sc1, DGs[tau], b_DG[tau]

                        def dg_unit(j=j, DG=DG, b_dg=b_dg):
                            for h in range(4):
                                S.op("pool", OP("tensor_scalar", DG[:, h, :], IDB[:, :], SG[:, j, h:h + 1], 1.0,
                                               ALU.mult, ALU.mult), reads=[b_SG, b_const], writes=[b_dg])
                        U.append(dg_unit)
                        for c in range(nch):
                            ri = c % 2
                            for h in range(4):
                                def lg_unit(c=c, j=j, h=h, ri=ri):
                                    pr = (h % 2) * 64
                                    bk = 6 + lrot[0] % 2
                                    lrot[0] += 1
                                    S.op("pe", OP("matmul", PS[bk][:, :], IQT[pr:pr + 64, h // 2, j * 128:(j + 1) * 128],
                                                  IKT[pr:pr + 64, c * 512:(c + 1) * 512], start=True, stop=True),
                                         reads=[b_IQT, b_IKT], writes=[b_PS[bk]])
                                    S.op("act", OP("activation", RR[ri][h][:, :], PS[bk][:, :], AF.Relu,
                                                   scale=AW[:, j, h:h + 1]),
                                         reads=[b_PS[bk], b_AW], writes=[b_RR[ri][h]])
                                U.append(lg_unit)

                            def cmb_unit(c=c, tau=tau, DG=DG, b_dg=b_dg, ri=ri):
                                bk = 6 + lrot[0] % 2
                                lrot[0] += 1
                                for h in range(4):
                                    S.op("pe", OP("matmul", PS[bk][:, :], DG[:, h, :], RR[ri][h][:, :],
                                                  start=(h == 0), stop=(h == 3)),
                                         reads=[b_dg, b_RR[ri][h]], writes=[b_PS[bk]], sig=(h == 3))
                                if c == nch - 1:
                                    S.op("dve", OP("tensor_tensor", SC1[:, c * 512:(c + 1) * 512], PS[bk][:, :],
                                                   ADM[:, tau, :], ALU.add),
                                         reads=[b_PS[bk], b_const], writes=[b_sc1])
                                else:
                                    S.op("act", OP("activation", SC1[:, c * 512:(c + 1) * 512], PS[bk][:, :], AF.Copy),
                                         reads=[b_PS[bk]], writes=[b_sc1])
                            U.append(cmb_unit)
                        if j == 0:
                            U.append(lambda: S.op("dve", OP("memset", CBf[:, 0:1], -10000.0), writes=[b_CBf[0]]))
                        else:
                            U.append(lambda: S.op("dve", OP("memset", CBd[:, 0:1], 0.0), writes=[b_CBd]))
                            for k in range(1, BIS_ITERS + 1):
                                def bis_d(k=k):
                                    wk = BIS_R / (2.0 ** k)
                                    S.op("dve", OP("tensor_scalar", MB[:, 1, 0:N], SC1[:, 0:N], CBd[:, 0:1], None,
                                                   ALU.is_ge, ALU.add, accum_out=CBd[:, 1:2]),
                                         reads=[b_sc1, b_CBd], writes=[b_MB[1], b_CBd])
                                    S.op("dve", OP("tensor_scalar", CBd[:, 2:3], CBd[:, 1:2], 255.5, -0.5,
                                                   ALU.is_ge, ALU.add), reads=[b_CBd], writes=[b_CBd])
                                    S.op("dve", OP("scalar_tensor_tensor", CBd[:, 0:1], CBd[:, 2:3], 2.0 * wk,
                                                   CBd[:, 0:1], ALU.mult, ALU.add), reads=[b_CBd], writes=[b_CBd])
                                U.append(bis_d)
                            U.append(lambda tau=tau: S.op(
                                "dve", OP("tensor_scalar", CBf[:, tau:tau + 1], CBd[:, 0:1], -wK, None, ALU.add),
                                reads=[b_CBd], writes=[b_CBf[tau]]))
                        U.append(lambda tau=tau: S.op(
                            "dve", OP("tensor_scalar", MB[:, tau, 0:N], SC1[:, 0:N], CBf[:, tau:tau + 1], None,
                                      ALU.is_ge), reads=[b_sc1, b_CBf[tau]], writes=[b_MB[tau]]))
                    for k2 in range(nkb // 2):
                        def tr_unit(k2=k2):
                            bk = 6 + lrot[0] % 2
                            lrot[0] += 1
                            for ee in range(2):
                                kb = 2 * k2 + ee
                                for tau in range(2):
                                    S.op("pe", OP("matmul", PS[bk][:, ee * 256 + tau * 128:ee * 256 + (tau + 1) * 128],
                                                  MB[:, tau, kb * 128:(kb + 1) * 128], IDB[:, :], start=True,
                                                  stop=True),
                                         reads=[b_MB[tau], b_const], writes=[b_PS[bk]], sig=(ee == 1 and tau == 1))
                            S.op("act", OP("activation", MT[:, 2 * k2:2 * k2 + 2, :].rearrange("p a t -> p (a t)"),
                                           PS[bk][:, :], AF.Copy), reads=[b_PS[bk]], writes=[b_MT])
                        U.append(tr_unit)
                    return U

                def qbd_build(g):
                    t0 = 2 * g * 128
                    qb, bq = QBD[g % 2], b_QBD[g % 2]
                    for hp in range(4):
                        S.op("act", OP("activation", qb[0:64, hp, 0:256], QT[0:64, hp, t0:t0 + 256], AF.Copy),
                             reads=[b_QT], writes=[bq])
                        S.op("act", OP("activation", qb[64:128, hp, 256:512], QT[64:128, hp, t0:t0 + 256], AF.Copy),
                             reads=[b_QT], writes=[bq])

                def att_units(g):
                    nkb = 4 * (g + 1)
                    MT, b_MT = MTs[g % 2], b_MTs[g % 2]
                    qb, bq = QBD[g % 2], b_QBD[g % 2]
                    units = [(hp, kb) for hp in range(4) for kb in range(nkb)]

                    def stage12(i):
                        hp, kb = units[i]
                        bs, pi = i % 2, i % 4
                        S.op("pe", OP("matmul", PS[bs][:, :], KT[:, hp, kb * 128:(kb + 1) * 128], qb[:, hp, :],
                                      start=True, stop=True),
                             reads=[b_KT, bq], writes=[b_PS[bs]])
                        S.op("act", OP("activation", PT[pi][:, :], PS[bs][:, :], AF.Exp, scale=0.125),
                             reads=[b_PS[bs]], writes=[b_PT[pi]])
                        S.op("pool", OP("tensor_tensor", PT[pi][:, :].rearrange("p (a t) -> p a t", a=2),
                                        PT[pi][:, :].rearrange("p (a t) -> p a t", a=2),
                                        MT[:, kb:kb + 1, :].to_broadcast([128, 2, 256]), ALU.mult),
                             reads=[b_PT[pi], b_MT], writes=[b_PT[pi]])

                    def stage3(i):
                        hp, kb = units[i]
                        pi = i % 4
                        bo, br = 2 + hp % 2, 4 + hp % 2
                        S.op("pe", OP("matmul", PS[bo][:, :], VA[:, kb, hp * 128:(hp + 1) * 128], PT[pi][:, :],
                                      start=(kb == 0), stop=(kb == nkb - 1)),
                             reads=[b_VA, b_PT[pi]], writes=[b_PS[bo]], sig=(kb == nkb - 1))
                        S.op("pe", OP("matmul", PS[br][:, :], ONES[:, :], PT[pi][:, :],
                                      start=(kb == 0), stop=(kb == nkb - 1)),
                             reads=[b_const, b_PT[pi]], writes=[b_PS[br]])
                    return units, stage12, stage3

                def att_norm(g, hp):
                    t0 = 2 * g * 128
                    bo, br = 2 + hp % 2, 4 + hp % 2
                    ri = hp % 2
                    for e2 in range(2):
                        pr, c0 = e2 * 64, e2 * 256
                        S.op("dve", OP("reciprocal", RC[ri][pr:pr + 64, c0:c0 + 256], PS[br][pr:pr + 64, c0:c0 + 256]),
                             reads=[b_PS[br]], writes=[b_RC[ri]])
                        S.op("dve", OP("tensor_tensor", AT[pr:pr + 64, hp, t0:t0 + 256], PS[bo][pr:pr + 64, c0:c0 + 256],
                                       RC[ri][pr:pr + 64, c0:c0 + 256], ALU.mult),
                             reads=[b_PS[bo], b_RC[ri]], writes=[b_AT])

                for u in ib_units(0):
                    u()
                qbd_build(0)
                for g in range(8):
                    ibu = ib_units(g + 1) if g < 7 else []
                    n = len(ibu)
                    units, stage12, stage3 = att_units(g)
                    U = len(units)
                    per_h = U // 4
                    ib_done = 0
                    SK = 2
                    for i in range(U + SK):
                        if i < U:
                            stage12(i)
                        if i >= SK:
                            i3 = i - SK
                            stage3(i3)
                            tgt = min(n, ((i3 + 1) * n * 5 + 3 * U - 1) // (3 * U))
                            for u in ibu[ib_done:tgt]:
                                u()
                            ib_done = tgt
                            hprev, kbprev = units[i3]
                            if kbprev == per_h - 1:
                                att_norm(g, hprev)
                            if conv_th and (i3 % 6 == 0):
                                conv_th.pop(0)()
                        if i == U // 2 and g < 7:
                            qbd_build(g + 1)
                    for u in ibu[ib_done:]:
                        u()
                while conv_th:
                    conv_th.pop(0)()
                if debug and l == 0:
                    S.dma("sp", dbg["d_at"].ap().rearrange("(c p) t -> p c t", p=128), AT[:, :, :], reads=[b_AT],
                          writes=[b_dbg], append=True)
                S.barrier()
                S.flush(block)

            ab.close()
            if stop_after == f"B{l}":
                return nc
            with ExitStack() as st, nc.Block() as block:
                XT = sb(st, "XTc", [128, 4, D], F32)
                GF = sb(st, "GF", [128, D], F32)
                b_GF = Buf("GF")
                if last:
                    S.dma("sp", GF[:, :], n_fin.ap().partition_broadcast(128), writes=[b_GF])
                UTgL = [sb(st, f"UTc{i}", [128, 8, 512], BF16) for i in range(2)]
                U2 = sb(st, "U2", [128, 8, 512], BF16)
                CHBL = [sb(st, f"CHB{i}", [128, 4, 4, 130], BF16) for i in range(2)]
                HAL = [sb(st, f"HA{i}", [128, 4, 4, 2], BF16) for i in range(2)]
                HBL = [sb(st, f"HB{i}", [128, 4, 4, 2], BF16) for i in range(2)]
                b_UTgL, b_CHBL = [Buf("UTc0"), Buf("UTc1")], [Buf("CHB0"), Buf("CHB1")]
                b_HAL, b_HBL = [Buf("HA0"), Buf("HA1")], [Buf("HB0"), Buf("HB1")]
                HT1 = sb(st, "HT1", [128, 4, 4, 2], F32)
                CV = sb(st, "CV", [128, 4, 128], F32)
                ZT = sb(st, "ZT", [128, 4, 512], BF16)
                SG0 = sb(st, "SG0", [128, 512], F32)
                SG1 = sb(st, "SG1", [128, 512], F32)
                M1 = sb(st, "M1", [128, 512], F32)
                M2 = sb(st, "M2", [128, 512], F32)
                MG = sb(st, "MG", [128, 8, 512], BF16)
                HT = sb(st, "HT", [128, 32, 512], BF16)
                RL = [sb(st, f"RL{i}", [128, 512], F32) for i in range(2)]
                XS2 = [sb(st, f"XS2c{i}", [128, D], F32) for i in range(2)]
                SSQ = sb(st, "SSQc", [128, 16], F32)
                b_x2c, b_ssc = [Buf("x2c0"), Buf("x2c1")], Buf("ssc")
                b_XT, b_UTg, b_U2, b_CHB, b_HA, b_HB, b_HT1, b_CV, b_ZT = (Buf(n) for n in (
                    "XTc", "UTc", "U2", "CHB", "HA", "HB", "HT1", "CV", "ZT"))
                b_SG0, b_SG1, b_M1, b_M2, b_MG, b_HT, b_nt, b_OT = (Buf(n) for n in (
                    "SG0", "SG1", "M1", "M2", "MG", "HT", "ntc", "OT"))
                b_RL = [Buf("RL0"), Buf("RL1")]
                loads = []
                for G in range(4):
                    loads += [(wsrc(WinB[l], C_CB, 512), 8, 512, b_W[("in", l)])]
                    for fh in range(2):
                        loads += [(wsrc(WaoB[l], fh * 512, 512, 0, 4), 4, 512, b_W[("ao", l)]),
                                  (wsrc(WcoB[l], fh * 512, 512, 0, 4), 4, 512, b_W[("co", l)]),
                                  (wsrc(WinB[l], C_G + fh * 512, 512), 8, 512, b_W[("in", l)]),
                                  (wsrc(WinB[l], C_G + 1024 + fh * 512, 512), 8, 512, b_W[("in", l)])]
                    loads += [(wsrc(WmxB[l], hf * 512, 512), 8, 512, b_W[("mx", l)]) for hf in range(2)]
                    loads += [(wsrc(WupB[l], i * 512, 512), 8, 512, b_W[("up", l)]) for i in range(8)]
                    loads += [(wsrc(WdnB[l], hf * 512, 512, hg * 8, 8), 8, 512, b_W[("dn", l)])
                              for hf in range(2) for hg in range(4)]
                NLG = len(loads) // 4
                ws = WStream(st, 7, loads, 4)
                xgp = XGp[l]
                chv = CHd.ap().rearrange("(c p) (j w) -> p c j w", p=128, w=130)

                def halo_view(pp):
                    base = pp * 264 + 256
                    return xgp[3][base:base + 8, :].rearrange("r (a b) -> (r a) b", b=32).rearrange(
                        "(c p) (j w) -> p c j w", p=128, w=2)

                def prefetch(G):
                    g0_ = G * 512
                    k_ = G % 2
                    S.dma("sp", UTgL[k_][:, :, :], UTd.ap().rearrange("(k p) t -> p k t", p=128)[:, :, g0_:g0_ + 512],
                          reads=[b_UTd], writes=[b_UTgL[k_]])
                    for c in range(4):
                        S.dma("sp", CHBL[k_][:, c, :, 2:130], chv[:, c, G * 4:G * 4 + 4, 2:130], reads=[b_CHd],
                              writes=[b_CHBL[k_]], append=(c > 0))
                    for c in range(4):
                        S.dma("sp", HAL[k_][:, c, :, :], halo_view(0)[:, c, G * 4:G * 4 + 4, :], reads=[b_XGd[l]],
                              writes=[b_HAL[k_]], append=(c > 0))
                    S.op("pool", OP("memset", HBL[k_][:, :, :, :], 0.0), writes=[b_HBL[k_]])
                    for c in range(4):
                        if G == 0:
                            S.dma("sp", HBL[k_][:, c, 1:4, :], halo_view(1)[:, c, 0:3, :], reads=[b_XGd[l]],
                                  writes=[b_HBL[k_]], append=True)
                        else:
                            S.dma("sp", HBL[k_][:, c, :, :], halo_view(1)[:, c, G * 4 - 1:G * 4 + 3, :],
                                  reads=[b_XGd[l]], writes=[b_HBL[k_]], append=True)

                prefetch(0)
                for G in range(4):
                    g0 = G * 512
                    wi = G * NLG
                    UTg, b_UTg, CHB, b_CHB = UTgL[G % 2], b_UTgL[G % 2], CHBL[G % 2], b_CHBL[G % 2]
                    HA, b_HA, HB, b_HB = HAL[G % 2], b_HAL[G % 2], HBL[G % 2], b_HBL[G % 2]
                    S.dma("sp", XT[:, :, :], x_src[g0:g0 + 512, :].rearrange("(t p) d -> p t d", p=128),
                          reads=[b_xsrc], writes=[b_XT])
                    S.op("dve", OP("tensor_scalar", HT1[:, :, :, :], HA[:, :, :, :], SEL[:, 0:1], None, ALU.mult),
                         reads=[b_HA, b_const], writes=[b_HT1])
                    S.op("dve", OP("scalar_tensor_tensor",
                        CHB[:, :, :, 0:2].rearrange("p c j w -> p (c j) w"),
                        HB[:, :, :, :].rearrange("p c j w -> p (c j) w"), SEL[:, 1:2],
                        HT1[:, :, :, :].rearrange("p c j w -> p (c j) w"), ALU.mult, ALU.add),
                         reads=[b_HB, b_HT1, b_const, b_CHB], writes=[b_CHB])
                    Wt, b_w = ws.get(wi + 0)
                    for c in range(4):
                        bk = c % 2
                        for kc in range(8):
                            S.op("pe", OP("matmul",
                                PS[bk][:, :], Wt[:, kc, c * 128:(c + 1) * 128], UTg[:, kc, :], start=(kc == 0),
                                stop=(kc == 7)), reads=[b_w, b_UTg], writes=[b_PS[bk]], sig=(kc == 7))
                        S.op("dve", OP("tensor_scalar", CV[:, :, :], CHB[:, c, :, 2:130], CW[:, l, c, 2:3],
                                                                   None, ALU.mult),
                             reads=[b_CHB, b_const], writes=[b_CV])
                        for jt in (1, 0):
                            S.op("dve", OP("scalar_tensor_tensor",
                                CV[:, :, :], CHB[:, c, :, jt:jt + 128], CW[:, l, c, jt:jt + 1], CV[:, :, :], ALU.mult,
                                ALU.add), reads=[b_CHB, b_const, b_CV], writes=[b_CV])
                        S.op("dve", OP("tensor_tensor",
                            ZT[:, c, :], PS[bk][:, :], CV[:, :, :].rearrange("p t w -> p (t w)"), ALU.mult),
                             reads=[b_PS[bk], b_CV], writes=[b_ZT])
                    for f in range(8):
                        if f % 4 == 0:
                            fh = f // 4
                            Wa, b_wa = ws.get(wi + 1 + 4 * fh)
                            Wc, b_wc = ws.get(wi + 2 + 4 * fh)
                            Wg0, b_wg0 = ws.get(wi + 3 + 4 * fh)
                            Wg1, b_wg1 = ws.get(wi + 4 + 4 * fh)
                        fo = (f % 4) * 128
                        ba, bc_, bg0, bg1 = (0, 1, 2, 3) if f % 2 == 0 else (4, 5, 6, 7)
                        for kc in range(4):
                            S.op("pe", OP("matmul",
                                PS[ba][:, :], Wa[:, kc, fo:fo + 128], AT[:, kc, g0:g0 + 512],
                                start=(kc == 0), stop=(kc == 3)), reads=[b_wa, b_AT], writes=[b_PS[ba]],
                                 sig=(kc == 3))
                        for kc in range(4):
                            S.op("pe", OP("matmul",
                                PS[bc_][:, :], Wc[:, kc, fo:fo + 128], ZT[:, kc, :],
                                start=(kc == 0), stop=(kc == 3)), reads=[b_wc, b_ZT], writes=[b_PS[bc_]],
                                 sig=(kc == 3))
                        for (Wg, b_wg, bg) in ((Wg0, b_wg0, bg0), (Wg1, b_wg1, bg1)):
                            for kc in range(8):
                                S.op("pe", OP("matmul",
                                    PS[bg][:, :], Wg[:, kc, fo:fo + 128], UTg[:, kc, :], start=(kc == 0),
                                    stop=(kc == 7)), reads=[b_wg, b_UTg], writes=[b_PS[bg]], sig=(kc == 7))
                        S.op("act", OP("activation", SG0[:, :], PS[bg0][:, :], AF.Sigmoid),
                             reads=[b_PS[bg0]], writes=[b_SG0])
                        S.op("act", OP("activation", SG1[:, :], PS[bg1][:, :], AF.Sigmoid),
                             reads=[b_PS[bg1]], writes=[b_SG1])
                        S.op("dve", OP("tensor_tensor", M1[:, :], PS[ba][:, :], SG0[:, :], ALU.mult),
                             reads=[b_PS[ba], b_SG0], writes=[b_M1])
                        S.op("dve", OP("tensor_tensor", M2[:, :], PS[bc_][:, :], SG1[:, :], ALU.mult),
                             reads=[b_PS[bc_], b_SG1], writes=[b_M2])
                        S.op("pool", OP("tensor_tensor", MG[:, f, :], M1[:, :], M2[:, :], ALU.add),
                             reads=[b_M1, b_M2], writes=[b_MG])
                    for hf in range(2):
                        Wm, b_wm = ws.get(wi + 9 + hf)
                        for tau in range(4):
                            bk = (hf * 4 + tau) % 4
                            for kc in range(8):
                                S.op("pe", OP("matmul",
                                    PS[bk][:, :], MG[:, kc, tau * 128:(tau + 1) * 128], Wm[:, kc, :],
                                    start=(kc == 0), stop=(kc == 7)), reads=[b_wm, b_MG], writes=[b_PS[bk]],
                                     sig=(kc == 7))
                            S.op("dve", OP("tensor_tensor",
                                XT[:, tau, hf * 512:(hf + 1) * 512], PS[bk][:, :], XT[:, tau, hf * 512:(hf + 1) * 512],
                                ALU.add), reads=[b_PS[bk], b_XT], writes=[b_XT])
                    if G < 3:
                        prefetch(G + 1)
                    norm_group((XS2, SSQ, b_x2c, b_ssc), XT, b_XT, GP[:, l, :], U2, b_U2)
                    for hc in range(32):
                        if hc % 4 == 0:
                            Wu, b_wu = ws.get(wi + 11 + hc // 4)
                        bk = hc % 4
                        ho = (hc % 4) * 128
                        for kc in range(8):
                            S.op("pe", OP("matmul",
                                PS[bk][:, :], Wu[:, kc, ho:ho + 128], U2[:, kc, :], start=(kc == 0), stop=(kc == 7)),
                                 reads=[b_wu, b_U2], writes=[b_PS[bk]], sig=(kc == 7))
                        S.op("act", OP("activation", RL[hc % 2][:, :], PS[bk][:, :], AF.Relu),
                             reads=[b_PS[bk]], writes=[b_RL[hc % 2]])
                        S.op("pool", OP("tensor_tensor", HT[:, hc, :], RL[hc % 2][:, :], RL[hc % 2][:, :],
                                                                      ALU.mult),
                             reads=[b_RL[hc % 2]], writes=[b_HT])
                    for hf in range(2):
                        for hg in range(4):
                            Wd, b_wd = ws.get(wi + 19 + hf * 4 + hg)
                            for tau in range(4):
                                bk = 4 + tau
                                for k8 in range(8):
                                    hc = hg * 8 + k8
                                    S.op("pe", OP("matmul",
                                        PS[bk][:, :], HT[:, hc, tau * 128:(tau + 1) * 128], Wd[:, k8, :],
                                        start=(hc == 0), stop=(hc == 31)), reads=[b_wd, b_HT], writes=[b_PS[bk]],
                                         sig=(k8 == 7))
                        for tau in range(4):
                            bk = 4 + tau
                            S.op("dve", OP("tensor_tensor",
                                XT[:, tau, hf * 512:(hf + 1) * 512], PS[bk][:, :], XT[:, tau, hf * 512:(hf + 1) * 512],
                                ALU.add), reads=[b_PS[bk], b_XT], writes=[b_XT])
                    if not last:
                        S.dma("sp", XRd[g0:g0 + 512, :].rearrange("(t p) d -> p t d", p=128), XT[:, :, :],
                              reads=[b_XT], writes=[b_XRd], append=True)
                    else:
                        for tau in range(4):
                            S.op("act", OP("activation", XS2[tau % 2][:, :], XT[:, tau, :], AF.Square,
                                           accum_out=SSQ[:, tau:tau + 1]), reads=[b_XT],
                                 writes=[b_x2c[tau % 2], b_ssc])
                        S.op("dve", OP("tensor_scalar", SSQ[:, 4:8], SSQ[:, 0:4], 1.0 / D, EPS, ALU.mult, ALU.add),
                             reads=[b_ssc], writes=[b_ssc])
                        S.op("act", OP("activation", SSQ[:, 8:12], SSQ[:, 4:8], AF.Sqrt), reads=[b_ssc],
                             writes=[b_ssc])
                        S.op("dve", OP("reciprocal", SSQ[:, 12:16], SSQ[:, 8:12]), reads=[b_ssc], writes=[b_ssc])
                        for tau in range(4):
                            ot, bo_ = XS2[tau % 2], b_x2c[tau % 2]
                            S.op("dve", OP("scalar_tensor_tensor", ot[:, :], XT[:, tau, :], SSQ[:, 12 + tau:13 + tau],
                                           GF[:, :], ALU.mult, ALU.mult),
                                 reads=[b_XT, b_ssc, b_GF], writes=[bo_])
                            S.dma("sp", out_d[g0 + tau * 128:g0 + (tau + 1) * 128, :], ot[:, :], reads=[bo_],
                                  writes=[b_out], append=True)
                S.barrier()
                S.flush(block)
    return nc


_CACHE = {}


def _rope_tables():
    inv = (1.0 / (np.float32(500000.0) ** (np.arange(0, 16, 2, dtype=np.float32) / np.float32(16)))).astype(np.float32)
    ang = (np.arange(T, dtype=np.float32)[:, None] * inv[None, :]).astype(np.float32)
    return np.cos(ang).astype(np.float32), np.sin(ang).astype(np.float32)


def _core_inputs(inputs, debug=False):
    cos, sin = _rope_tables()
    ident = np.eye(128, dtype=np.float32)
    shared = {k: np.ascontiguousarray(np.asarray(inputs[k], dtype=np.float32)) for k in
              ("w_in", "w_attn_out", "w_conv_out", "w_mix_out", "w_mlp_up", "w_mlp_down", "norm_mix", "norm_mlp",
               "norm_final", "conv_w")}
    x = np.asarray(inputs["x"], dtype=np.float32)
    maps = []
    for core in range(NCORES):
        b, p = core // 2, core % 2
        tiles = np.arange(NT) * 2 + p
        tok = (tiles[:, None] * 128 + np.arange(128)[None, :]).reshape(-1)
        xc = np.ascontiguousarray(x[b][tok])
        c = cos[tok].T
        s = sin[tok].T
        ropeC = np.ascontiguousarray(np.concatenate([c, c], 0))
        ropeS = np.ascontiguousarray(np.concatenate([-s, s], 0))
        adm = np.zeros((128, 2, 512), np.float32)
        col = np.arange(512)[None, :]
        tt = np.arange(128)[:, None]
        for tau in range(2):
            lim = 128 * (2 * tau + p) + 64 + 64 * (tt >= 64)
            adm[:, tau, :] = np.where(col < lim, 0.0, NEG)
        sel = np.zeros((128, 2), np.float32)
        sel[:, 0] = 1.0 if p == 1 else 0.0
        sel[:, 1] = 1.0 if p == 0 else 0.0
        m = dict(shared)
        m.update({"x": xc, "ropeC": ropeC, "ropeS": ropeS, "adm": adm, "sel": sel, "ident": ident})
        maps.append(m)
    return maps


def kernel(**inputs):
    if "nc" not in _CACHE:
        _CACHE["nc"] = build_program()
    nc = _CACHE["nc"]
    maps = _core_inputs(inputs)
    res = run_bass_kernel_spmd(nc, maps, core_ids=list(range(NCORES)))
    out = np.empty((NB, T, D), np.float32)
    for core in range(NCORES):
        b, p = core // 2, core % 2
        o = np.asarray(res.results[core]["out"], dtype=np.float32).reshape(NT, 128, D)
        for j in range(NT):
            m = 2 * j + p
            out[b, m * 128:(m + 1) * 128, :] = o[j]
    return out
```

```python
import os
import numpy as np
from contextlib import ExitStack
import concourse.bass as bass
import concourse.mybir as mybir
from concourse.bass_utils import run_bass_kernel_spmd

F32 = mybir.dt.float32
BF16 = mybir.dt.bfloat16
AF = mybir.ActivationFunctionType
ALU = mybir.AluOpType

D = 1024
T = 4096
NB = 4
DEPTH = 2
NCORES = 8
TL = 2048
NT = 16
INC = 5444
C_Q, C_K, C_V, C_IQ, C_IK, C_IW, C_CB, C_CC, C_CH, C_G = 0, 512, 1024, 1536, 1792, 1856, 1860, 2372, 2884, 3396
HID = 4096
EPS = 1e-6
XS_ROWS = 1096
NEG = -30000.0
BIS_ITERS = 10
BIS_R = 4.0
ACT_BISECT = False
SAME_ENG_SYNC = True
NDSEM = 32
NCONV = 48
CUT = int(os.environ.get('K_CUT', '99'))


def OP(name, *a, **k):
    return (name, a, k)


class Buf:
    __slots__ = ("name", "w", "r")

    def __init__(self, name):
        self.name = name
        self.w = []
        self.r = {}


class Sched:
    def __init__(self, nc, sems, dsems):
        self.nc = nc
        self.q = {k: [] for k in ("pe", "act", "dve", "pool", "sp")}
        self.cnt = {k: 0 for k in self.q}
        self.cnt["cc"] = 0
        self.known = {k: {} for k in self.q}
        self.sems = sems
        self.dsems = dsems
        self.dcnt = [0] * len(dsems)
        self.dnext = 0
        self.mute = False

    def _wait(self, eng, tok):
        kind, key, val = tok
        if kind == "e" and key == eng and (eng == "pe" or not SAME_ENG_SYNC):
            return
        k = (kind, key)
        if self.known[eng].get(k, 0) >= val:
            return
        self.known[eng][k] = val
        sem = self.sems[key] if kind == "e" else self.dsems[key]
        self.q[eng].append(OP("wait_ge", sem, val))

    def _deps(self, eng, reads, writes):
        for b in reads:
            for t in b.w:
                self._wait(eng, t)
        for b in writes:
            for t in b.w:
                self._wait(eng, t)
            for t in b.r.values():
                self._wait(eng, t)

    def _mark(self, tok, reads, writes, append=False):
        for b in reads:
            b.r[(tok[0], tok[1])] = tok
        for b in writes:
            if append:
                b.w.append(tok)
            else:
                b.w = [tok]
                b.r = {}

    def op(self, eng, fn, reads=(), writes=(), sig=True):
        if self.mute:
            return
        self._deps(eng, reads, writes)
        if sig:
            self.cnt[eng] += 1
            tok = ("e", eng, self.cnt[eng])
            sem = self.sems[eng]
            self.q[eng].append(lambda e, fn=fn, sem=sem: getattr(e, fn[0])(*fn[1], **fn[2]).then_inc(sem, 1))
        else:
            tok = ("e", eng, self.cnt[eng] + 1)
            self.q[eng].append(lambda e, fn=fn: getattr(e, fn[0])(*fn[1], **fn[2]))
        self._mark(tok, reads, writes)

    def dma(self, qeng, out_ap, in_ap, reads=(), writes=(), append=False, fixed=None, **kw):
        if self.mute:
            return
        self._deps(qeng, reads, writes)
        if fixed is not None:
            i = NDSEM + fixed
        else:
            i = self.dnext
            self.dnext = (i + 1) % NDSEM
        if self.dcnt[i] > 0:
            self._wait(qeng, ("d", i, 16 * self.dcnt[i]))
        self.dcnt[i] += 1
        tok = ("d", i, 16 * self.dcnt[i])
        sem = self.dsems[i]
        self.q[qeng].append(
            lambda e, o=out_ap, a=in_ap, sem=sem, kw=kw: e.dma_start(out=o, in_=a, **kw).then_inc(sem, 16))
        self._mark(tok, reads, writes, append=append)

    def collective(self, ins_ap, outs_ap, reads, writes):
        if self.mute:
            return
        eng = "pool"
        self._deps(eng, reads, writes)
        self.cnt["cc"] += 1
        tok = ("e", "cc", self.cnt["cc"])
        sem = self.sems["cc"]
        groups = [[0, 1], [2, 3], [4, 5], [6, 7]]
        self.q[eng].append(
            lambda e, a=ins_ap, o=outs_ap, sem=sem: e.collective_compute(
                "AllGather", ALU.bypass, replica_groups=groups, ins=[a], outs=[o]).then_inc(sem))
        self._mark(tok, reads, writes)

    def barrier(self):
        toks = [("e", k, self.cnt[k]) for k in ("pe", "act", "dve", "pool", "sp", "cc") if self.cnt[k] > 0]
        toks += [("d", i, 16 * c) for i, c in enumerate(self.dcnt) if c > 0]
        for eng in self.q:
            for t in toks:
                if t[0] == "e" and t[1] == eng:
                    continue
                self._wait(eng, t)

    def flush(self, block):
        q = self.q

        def run(lst, e):
            for f in lst:
                if isinstance(f, tuple):
                    getattr(e, f[0])(*f[1], **f[2])
                else:
                    f(e)

        @block.tensor
        def _(e):
            run(q["pe"], e)

        @block.scalar
        def _(e):
            run(q["act"], e)

        @block.vector
        def _(e):
            run(q["dve"], e)

        @block.gpsimd
        def _(e):
            run(q["pool"], e)

        @block.sync
        def _(e):
            run(q["sp"], e)

        self.q = {k: [] for k in q}


def build_program(debug=False, nlayers=DEPTH, stop_after=None):
    nc = bass.Bass("TRN2", target_bir_lowering=False)
    dt = nc.dram_tensor
    x_in = dt("x", [TL, D], F32, kind="ExternalInput")
    w_in = dt("w_in", [DEPTH, D, INC], F32, kind="ExternalInput")
    w_ao = dt("w_attn_out", [DEPTH, 512, D], F32, kind="ExternalInput")
    w_co = dt("w_conv_out", [DEPTH, 512, D], F32, kind="ExternalInput")
    w_mx = dt("w_mix_out", [DEPTH, D, D], F32, kind="ExternalInput")
    w_up = dt("w_mlp_up", [DEPTH, D, HID], F32, kind="ExternalInput")
    w_dn = dt("w_mlp_down", [DEPTH, HID, D], F32, kind="ExternalInput")
    n_mix = dt("norm_mix", [DEPTH, D], F32, kind="ExternalInput")
    n_mlp = dt("norm_mlp", [DEPTH, D], F32, kind="ExternalInput")
    n_fin = dt("norm_final", [D], F32, kind="ExternalInput")
    conv_w = dt("conv_w", [DEPTH, 3, 512], F32, kind="ExternalInput")
    ropeC = dt("ropeC", [16, TL], F32, kind="ExternalInput")
    ropeS = dt("ropeS", [16, TL], F32, kind="ExternalInput")
    adm_in = dt("adm", [128, 2, 512], F32, kind="ExternalInput")
    sel_in = dt("sel", [128, 2], F32, kind="ExternalInput")
    ident_in = dt("ident", [128, 128], F32, kind="ExternalInput")
    out_d = dt("out", [TL, D], F32, kind="ExternalOutput")
    dbg = {}
    if debug:
        for nm, shp, ty in [("d_ut", [D, TL], BF16), ("d_xs", [XS_ROWS, TL], BF16), ("d_qt", [512, TL], BF16),
                            ("d_iqt", [256, TL], BF16), ("d_at", [512, TL], BF16), ("d_aw", [128, 64], F32),
                            ("d_sg", [128, 64], F32), ("d_sc", [128, 4096], F32), ("d_mb", [128, 8192], BF16),
                            ("d_cb", [128, 1], F32)]:
            dbg[nm] = dt(nm, shp, ty, kind="ExternalOutput")

    WinB = [dt(f"WinB{l}", [D, INC], BF16) for l in range(DEPTH)]
    WaoB = [dt(f"WaoB{l}", [512, D], BF16) for l in range(DEPTH)]
    WcoB = [dt(f"WcoB{l}", [512, D], BF16) for l in range(DEPTH)]
    WmxB = [dt(f"WmxB{l}", [D, D], BF16) for l in range(DEPTH)]
    WupB = [dt(f"WupB{l}", [D, HID], BF16) for l in range(DEPTH)]
    WdnB = [dt(f"WdnB{l}", [HID, D], BF16) for l in range(DEPTH)]
    UTd = dt("UTd", [D, TL], BF16)
    CHd = dt("CHd", [512, NT * 130], BF16)
    XRd = dt("XRd", [TL, D], F32)
    PR = [256, 320, 256, 264]
    XSp = [[dt(f"XSp{l}_{k}", [PR[k], TL], BF16) for k in range(4)] for l in range(DEPTH)]
    XGp = [[dt(f"XGp{l}_{k}", [2 * PR[k], TL], BF16) for k in range(4)] for l in range(DEPTH)]

    b_W = {(nm, l): Buf(f"{nm}{l}") for nm in ("in", "ao", "co", "mx", "up", "dn") for l in range(DEPTH)}
    b_UTd, b_CHd, b_XRd = Buf("UTd"), Buf("CHd"), Buf("XRd")
    b_XSd = [Buf("XSd0"), Buf("XSd1")]
    b_XGd = [Buf("XGd0"), Buf("XGd1")]
    b_out = Buf("out")
    b_dbg = Buf("dbg")

    with ExitStack() as top:
        sems = {k: top.enter_context(nc.semaphore(f"s_{k}")) for k in ("pe", "act", "dve", "pool", "sp", "cc")}
        dsems = [top.enter_context(nc.semaphore(f"d_{i}")) for i in range(NDSEM + NCONV)]
        S = Sched(nc, sems, dsems)

        uid = [0]

        def sb(st, name, shape, dtype):
            uid[0] += 1
            return st.enter_context(nc.sbuf_tensor(f"{name}_{uid[0]}", shape, dtype))

        AT = sb(top, "AT", [128, 4, TL], BF16)
        AW = sb(top, "AW", [128, NT, 4], F32)
        SG = sb(top, "SG", [128, NT, 4], F32)
        IDF = sb(top, "IDF", [128, 128], F32)
        IDB = sb(top, "IDB", [128, 128], BF16)
        ONES = sb(top, "ONES", [128, 128], BF16)
        ADM = sb(top, "ADM", [128, 2, 512], F32)
        SEL = sb(top, "SEL", [128, 2], F32)
        GM = sb(top, "GM", [128, DEPTH, 8], F32)
        GP = sb(top, "GP", [128, DEPTH, 8], F32)
        CW = sb(top, "CW", [128, DEPTH, 4, 3], F32)
        b_QT, b_IQT, b_AT, b_AW, b_SG = Buf("QT"), Buf("IQT"), Buf("AT"), Buf("AW"), Buf("SG")
        b_const = Buf("const")
        PS = [top.enter_context(nc.psum_tensor(f"ps{i}", [128, 512], F32)) for i in range(8)]
        b_PS = [Buf(f"ps{i}") for i in range(8)]

        with nc.Block() as block:
            S.dma("sp", IDF[:, :], ident_in[:, :], writes=[b_const], append=True)
            S.dma("pool", IDB[:, :], ident_in[:, :], writes=[b_const], append=True, fixed=40)
            S.dma("sp", ADM[:, :, :], adm_in[:, :, :], writes=[b_const], append=True)
            S.dma("sp", SEL[:, :], sel_in[:, :], writes=[b_const], append=True)
            for l in range(DEPTH):
                S.dma("sp", GM[:, l, :], n_mix[l, :].rearrange("(k p) -> p k", p=128), writes=[b_const],
                      append=True, allow_slow_non_contiguous=True)
                S.dma("sp", GP[:, l, :], n_mlp[l, :].rearrange("(k p) -> p k", p=128), writes=[b_const],
                      append=True, allow_slow_non_contiguous=True)
                for jt in range(3):
                    S.dma("sp", CW[:, l, :, jt], conv_w[l, jt, :].rearrange("(c p) -> p c", p=128),
                          writes=[b_const], append=True, allow_slow_non_contiguous=True)
            S.op("pool", OP("memset", ONES[:, :], 1.0), writes=[b_const])
            with ExitStack() as st0:
                stg = [sb(st0, f"stg{i}", [128, 2048], F32) for i in range(4)]
                stb = [sb(st0, f"stb{i}", [128, 2048], BF16) for i in range(4)]
                b_stg = [Buf(f"stg{i}") for i in range(4)]
                b_stb = [Buf(f"stb{i}") for i in range(4)]
                tiles_ = [(r0, c0, n_) for r0 in range(0, D, 128) for (c0, n_) in ((0, C_CB), (C_CC, C_G - C_CC))]

                def ld(i):
                    r0, c0, n_ = tiles_[i]
                    S.dma("sp", stg[i % 4][:, 0:n_], w_in[0, r0:r0 + 128, c0:c0 + n_], writes=[b_stg[i % 4]])
                for i in range(4):
                    ld(i)
                for i, (r0, c0, n_) in enumerate(tiles_):
                    k = i % 4
                    if i % 2 == 0:
                        S.op("dve", OP("tensor_copy", stb[k][:, 0:n_], stg[k][:, 0:n_]), reads=[b_stg[k]],
                             writes=[b_stb[k]])
                    else:
                        S.op("act", OP("activation", stb[k][:, 0:n_], stg[k][:, 0:n_], AF.Copy), reads=[b_stg[k]],
                             writes=[b_stb[k]])
                    S.dma("sp", WinB[0][r0:r0 + 128, c0:c0 + n_], stb[k][:, 0:n_], reads=[b_stb[k]],
                          writes=[b_W[("in", 0)]], append=True)
                    if i + 4 < len(tiles_):
                        ld(i + 4)
            S.flush(block)

        if stop_after == "init":
            return nc
        def conversion_thunks():
            th = []
            ncv = 0
            for (c0_, c1_) in ((C_CB, C_CC), (C_G, INC)):
                for r0 in range(0, D, 512):
                    def cv0(c0_=c0_, c1_=c1_, r0=r0, ncv=ncv):
                        S.dma("pool", WinB[0][r0:r0 + 512, c0_:c1_], w_in[0, r0:r0 + 512, c0_:c1_],
                              writes=[b_W[("in", 0)]], append=True, fixed=ncv)
                    th.append(cv0)
                    ncv += 1
            for l_ in range(nlayers):
                for (nm, src, dst, rows, rb) in (("in", w_in, WinB, D, 256), ("ao", w_ao, WaoB, 512, 512),
                                                 ("co", w_co, WcoB, 512, 512), ("mx", w_mx, WmxB, D, 512),
                                                 ("up", w_up, WupB, D, 256), ("dn", w_dn, WdnB, HID, 512)):
                    if l_ == 0 and nm == "in":
                        continue
                    for r0 in range(0, rows, rb):
                        def cv(l_=l_, nm=nm, src=src, dst=dst, r0=r0, rb=rb, ncv=ncv):
                            S.dma("pool",
                                  dst[l_][r0:r0 + rb, :].rearrange("r c -> (r c)").rearrange("(a b) -> a b", a=16),
                                  src[l_, r0:r0 + rb, :].rearrange("r c -> (r c)").rearrange("(a b) -> a b", a=16),
                                  writes=[b_W[(nm, l_)]], append=True, fixed=ncv)
                        th.append(cv)
                        ncv += 1
            return th

        conv_th = conversion_thunks()

        def norm_group(bufs, xt, b_xt, gvec_ap, dst, b_dst):
            XS2, SSQ, b_x2, b_ss = bufs
            for tau in range(4):
                S.op("act", OP("activation", XS2[tau % 2][:, :], xt[:, tau, :], AF.Square,
                               accum_out=SSQ[:, tau:tau + 1]), reads=[b_xt], writes=[b_x2[tau % 2], b_ss])
            S.op("dve", OP("tensor_scalar", SSQ[:, 4:8], SSQ[:, 0:4], 1.0 / D, EPS, ALU.mult, ALU.add),
                 reads=[b_ss], writes=[b_ss])
            S.op("act", OP("activation", SSQ[:, 8:12], SSQ[:, 4:8], AF.Sqrt), reads=[b_ss], writes=[b_ss])
            S.op("dve", OP("reciprocal", SSQ[:, 12:16], SSQ[:, 8:12]), reads=[b_ss], writes=[b_ss])
            for tau in range(4):
                x2, bx = XS2[tau % 2], b_x2[tau % 2]
                c0 = tau * 128
                S.op("dve", OP("tensor_scalar", x2[:, :], xt[:, tau, :], SSQ[:, 12 + tau:13 + tau], None, ALU.mult),
                     reads=[b_xt, b_ss], writes=[bx])
                for half in range(2):
                    bk = 6 + half
                    for k4 in range(4):
                        kc = half * 4 + k4
                        S.op("pe", OP("transpose", PS[bk][:, k4 * 128:(k4 + 1) * 128],
                                      x2[:, kc * 128:(kc + 1) * 128], IDF[:, :]),
                             reads=[bx, b_const], writes=[b_PS[bk]], sig=(k4 == 3))
                    for k4 in range(4):
                        kc = half * 4 + k4
                        S.op("act", OP("activation", dst[:, kc, c0:c0 + 128], PS[bk][:, k4 * 128:(k4 + 1) * 128],
                                       AF.Copy, scale=gvec_ap[:, kc:kc + 1]),
                             reads=[b_PS[bk], b_const], writes=[b_dst])

        class WStream:
            def __init__(self, st, nbuf, loads, live):
                self.ahead = nbuf - live
                self.bufs = [sb(st, f"WB{i}", [128, 4096], BF16) for i in range(nbuf)]
                self.bb = [Buf(f"WB{i}") for i in range(nbuf)]
                self.loads = loads
                self.issued = 0
                self.nbuf = nbuf

            def _issue(self):
                i = self.issued
                src, k, c, wbuf = self.loads[i]
                t = self.bufs[i % self.nbuf]
                dst = t[:, 0:k * c].rearrange("p (k c) -> p k c", k=k)
                S.dma("sp", dst, src, reads=[wbuf], writes=[self.bb[i % self.nbuf]])
                self.issued += 1

            def get(self, i):
                while self.issued < min(len(self.loads), i + self.ahead + 1):
                    self._issue()
                src, k, c, wbuf = self.loads[i]
                t = self.bufs[i % self.nbuf]
                return t[:, 0:k * c].rearrange("p (k c) -> p k c", k=k), self.bb[i % self.nbuf]

        def wsrc(tensor_l, c0, ncols, k0=0, nk=8):
            return tensor_l.ap().rearrange("(k p) c -> p k c", p=128)[:, k0:k0 + nk, c0:c0 + ncols]

        for l in range(nlayers):
            x_src, b_xsrc = (x_in, b_const) if l == 0 else (XRd, b_XRd)
            last = (l == nlayers - 1)
            ab = ExitStack()
            QT = sb(ab, "QT", [128, 4, TL], BF16)
            IQT = sb(ab, "IQT", [128, 2, TL], BF16)
            with ExitStack() as st, nc.Block() as block:
                CT = sb(st, "CT", [128, TL], F32)
                STb = sb(st, "STb", [128, TL], F32)
                XT = [sb(st, f"XTa{i}", [128, 4, D], F32) for i in range(2)]
                UTg = [sb(st, f"UTg{i}", [128, 8, 512], BF16) for i in range(2)]
                WSW = sb(st, "WSW", [128, 11, 8, 128], BF16)
                KS = sb(st, "KS", [128, 4, 512], BF16)
                IKS = sb(st, "IKS", [64, 512], BF16)
                VS = sb(st, "VS", [128, 4, 512], BF16)
                CS = [sb(st, f"CS{i}", [128, 512], F32) for i in range(2)]
                CHS = sb(st, "CHS", [128, 4, 4, 128], BF16)
                HS = sb(st, "HS", [128, 4, NT, 2], BF16)
                T1 = [sb(st, f"T1{i}", [128, 512], F32) for i in range(2)]
                T2 = [sb(st, f"T2{i}", [128, 512], F32) for i in range(2)]
                XS2 = [sb(st, f"XS2a{i}", [128, D], F32) for i in range(2)]
                SSQ = sb(st, "SSQ", [128, 16], F32)
                b_rope, b_XT, b_UTg, b_WSW = Buf("rope"), [Buf("XT0"), Buf("XT1")], [Buf("UTg0"), Buf("UTg1")], \
                    Buf("WSW")
                b_KS, b_IKS, b_VS, b_CS, b_CHS, b_HS = Buf("KS"), Buf("IKS"), Buf("VS"), [Buf("CS0"), Buf("CS1")], \
                    Buf("CHS"), Buf("HS")
                b_T1, b_T2 = [Buf("T10"), Buf("T11")], [Buf("T20"), Buf("T21")]
                b_x2a, b_ssa = [Buf("x2a0"), Buf("x2a1")], Buf("ssa")
                S.op("dve", OP("memset", CT[:, :], 1.0), writes=[b_rope])
                S.op("dve", OP("memset", STb[:, :], 0.0), writes=[b_rope])
                for r0 in (0, 64):
                    S.dma("sp", CT[r0:r0 + 16, :], ropeC[:, :], writes=[b_rope], append=True)
                    S.dma("sp", STb[r0:r0 + 16, :], ropeS[:, :], writes=[b_rope], append=True)
                loads = [(wsrc(WinB[l], C_Q, 512), 8, 512, b_W[("in", l)]),
                         (wsrc(WinB[l], C_K, 512), 8, 512, b_W[("in", l)]),
                         (wsrc(WinB[l], C_IQ, 324), 8, 324, b_W[("in", l)])]
                for G in range(4):
                    loads += [(wsrc(WinB[l], C_Q, 512), 8, 512, b_W[("in", l)]),
                              (wsrc(WinB[l], C_K, 512), 8, 512, b_W[("in", l)]),
                              (wsrc(WinB[l], C_V, 512), 8, 512, b_W[("in", l)]),
                              (wsrc(WinB[l], C_IQ, 324), 8, 324, b_W[("in", l)]),
                              (wsrc(WinB[l], C_CC, 512), 8, 512, b_W[("in", l)]),
                              (wsrc(WinB[l], C_CH, 512), 8, 512, b_W[("in", l)])]
                ws = WStream(st, 4, loads, 2)
                xsp = XSp[l]
                swi = [0]
                bank = [0]

                def fm_proj(Wt, b_w, c0, nf, utg, b_utg, bk):
                    for kc in range(8):
                        S.op("pe", OP("matmul", PS[bk][0:nf, :], Wt[:, kc, c0:c0 + nf], utg[:, kc, :],
                                                             start=(kc == 0), stop=(kc == 7)),
                             reads=[b_w, b_utg], writes=[b_PS[bk]], sig=(kc == 7))

                ci = 0
                for li, nch_, in ((0, 4), (1, 4), (2, 3)):
                    Wt0, b_w0 = ws.get(li)
                    for c in range(nch_):
                        nf = 64 if (li == 2 and c == 2) else 128
                        c0 = c * 128
                        S.op("dve", OP("tensor_copy", WSW[:, ci, :, 0:nf], Wt0[:, :, c0:c0 + nf]), reads=[b_w0],
                             writes=[b_WSW])
                        for hb in range(0, nf, 64):
                            S.op("dve", OP("tensor_copy", WSW[:, ci, :, hb:hb + 8], Wt0[:, :, c0 + hb + 8:c0 + hb + 16]),
                                 reads=[b_w0], writes=[b_WSW])
                            S.op("dve", OP("tensor_copy", WSW[:, ci, :, hb + 8:hb + 16], Wt0[:, :, c0 + hb:c0 + hb + 8]),
                                 reads=[b_w0], writes=[b_WSW])
                        ci += 1

                def rope_chunk(Wt, b_w, c0, nf, utg, b_utg, dst_ap, b_dst, g0, ci):
                    si = swi[0] % 2
                    swi[0] += 1
                    bq = bank[0] % 6
                    bs = (bank[0] + 1) % 6
                    bank[0] += 2
                    fm_proj(Wt, b_w, c0, nf, utg, b_utg, bq)
                    fm_proj(WSW[:, ci, :, :], b_WSW, 0, nf, utg, b_utg, bs)
                    S.op("dve", OP("tensor_tensor", T1[si][0:nf, :], PS[bs][0:nf, :], STb[0:nf, g0:g0 + 512], ALU.mult),
                         reads=[b_PS[bs], b_rope], writes=[b_T1[si]])
                    S.op("dve", OP("tensor_tensor", T2[si][0:nf, :], PS[bq][0:nf, :], CT[0:nf, g0:g0 + 512], ALU.mult),
                         reads=[b_PS[bq], b_rope], writes=[b_T2[si]])
                    S.op("dve", OP("tensor_tensor", dst_ap, T1[si][0:nf, :], T2[si][0:nf, :], ALU.add),
                         reads=[b_T1[si], b_T2[si]], writes=[b_dst])

                def xload(G_):
                    S.dma("sp", XT[G_ % 2][:, :, :],
                          x_src[G_ * 512:G_ * 512 + 512, :].rearrange("(t p) d -> p t d", p=128),
                          reads=[b_xsrc], writes=[b_XT[G_ % 2]])

                def head_norm(G_):
                    k_ = G_ % 2
                    norm_group((XS2, SSQ, b_x2a, b_ssa), XT[k_], b_XT[k_], GM[:, l, :], UTg[k_], b_UTg[k_])
                    S.dma("sp", UTd.ap().rearrange("(k p) t -> p k t", p=128)[:, :, G_ * 512:G_ * 512 + 512],
                          UTg[k_][:, :, :], reads=[b_UTg[k_]], writes=[b_UTd], append=True)

                xload(0)
                head_norm(0)
                for G in range(4):
                    g0 = G * 512
                    xb = G % 2
                    xt, utg = XT[xb], UTg[xb]
                    if G < 3:
                        xload(G + 1)
                    S.mute = CUT < 2
                    Wt, b_w = ws.get(3 + G * 6 + 0)
                    for c in range(4):
                        rope_chunk(Wt, b_w, c * 128, 128, utg, b_UTg[xb], QT[:, c, g0:g0 + 512], b_QT, g0, c)
                    S.mute = CUT < 3
                    Wt, b_w = ws.get(3 + G * 6 + 1)
                    for c in range(4):
                        rope_chunk(Wt, b_w, c * 128, 128, utg, b_UTg[xb], KS[:, c, :], b_KS, g0, 4 + c)
                    for hh in range(2):
                        S.dma("sp", xsp[hh][0:256, :].rearrange("(c p) t -> p c t", p=128)[:, :, g0:g0 + 512],
                              KS[:, 2 * hh:2 * hh + 2, :], reads=[b_KS], writes=[b_XSd[l]], append=True)
                    S.mute = CUT < 4
                    Wt, b_w = ws.get(3 + G * 6 + 2)
                    for tau in range(4):
                        bk = bank[0] % 6
                        bank[0] += 1
                        for kc in range(8):
                            S.op("pe", OP("matmul",
                                PS[bk][:, :], utg[:, kc, tau * 128:(tau + 1) * 128], Wt[:, kc, :],
                                start=(kc == 0), stop=(kc == 7)),
                                 reads=[b_w, b_UTg[xb]], writes=[b_PS[bk]], sig=(kc == 7))
                        S.op("act", OP("activation", VS[:, tau, :], PS[bk][:, :], AF.Copy),
                             reads=[b_PS[bk]], writes=[b_VS])
                    vview = xsp[2 + G // 2][0:256, :].rearrange("r (q c) -> (r q) c", q=4)
                    S.dma("sp", vview[(G % 2) * 512:(G % 2) * 512 + 512, :].rearrange("(t p) c -> p t c", p=128),
                          VS[:, :, :],
                          reads=[b_VS], writes=[b_XSd[l]], append=True)
                    S.mute = CUT < 5
                    Wt, b_w = ws.get(3 + G * 6 + 3)
                    for c in range(2):
                        rope_chunk(Wt, b_w, c * 128, 128, utg, b_UTg[xb], IQT[:, c, g0:g0 + 512], b_IQT, g0, 8 + c)
                    rope_chunk(Wt, b_w, 256, 64, utg, b_UTg[xb], IKS[:, :], b_IKS, g0, 10)
                    S.dma("sp", xsp[1][256:320, g0:g0 + 512], IKS[:, :], reads=[b_IKS], writes=[b_XSd[l]], append=True)
                    for tau in range(4):
                        j = G * 4 + tau
                        bk = bank[0] % 6
                        bank[0] += 1
                        for kc in range(8):
                            S.op("pe", OP("matmul",
                                PS[bk][:, 0:4], utg[:, kc, tau * 128:(tau + 1) * 128], Wt[:, kc, 320:324],
                                start=(kc == 0), stop=(kc == 7)),
                                 reads=[b_w, b_UTg[xb]], writes=[b_PS[bk]], sig=(kc == 7))
                        S.op("act", OP("activation", AW[:, j, :], PS[bk][:, 0:4], AF.Abs, scale=1.0 / 16.0),
                             reads=[b_PS[bk]], writes=[b_AW])
                        S.op("act", OP("activation", SG[:, j, :], PS[bk][:, 0:4], AF.Sign),
                             reads=[b_PS[bk]], writes=[b_SG])
                    if G < 3:
                        head_norm(G + 1)
                    S.mute = CUT < 6
                    WtC, b_wC = ws.get(3 + G * 6 + 4)
                    WtH, b_wH = ws.get(3 + G * 6 + 5)
                    for c in range(4):
                        bc = bank[0] % 6
                        bh = (bank[0] + 1) % 6
                        bank[0] += 2
                        fm_proj(WtC, b_wC, c * 128, 128, utg, b_UTg[xb], bc)
                        fm_proj(WtH, b_wH, c * 128, 128, utg, b_UTg[xb], bh)
                        S.op("act", OP("activation", CS[c % 2][:, :], PS[bc][:, :], AF.Copy),
                             reads=[b_PS[bc]], writes=[b_CS[c % 2]])
                        S.op("dve", OP("tensor_tensor",
                            CHS[:, c, :, :], CS[c % 2][:, :].rearrange("p (t w) -> p t w", t=4),
                            PS[bh][:, :].rearrange("p (t w) -> p t w", t=4), ALU.mult),
                             reads=[b_PS[bh], b_CS[c % 2]], writes=[b_CHS])
                    chv = CHd.ap().rearrange("(c p) (j w) -> p c j w", p=128, w=130)
                    for c in range(4):
                        S.dma("sp", chv[:, c, G * 4:G * 4 + 4, 2:130], CHS[:, c, :, :], reads=[b_CHS], writes=[b_CHd],
                              append=True)
                    S.op("dve", OP("tensor_copy", HS[:, :, G * 4:G * 4 + 4, :], CHS[:, :, :, 126:128]),
                         reads=[b_CHS], writes=[b_HS])
                S.mute = CUT < 7
                hv = xsp[3][256:264, :].rearrange("r (a b) -> (r a) b", b=32).rearrange("(c p) b -> p c b", p=128)
                S.dma("sp", hv, HS[:, :, :, :].rearrange("p c j w -> p c (j w)"), reads=[b_HS], writes=[b_XSd[l]],
                      append=True)
                S.mute = False
                if debug and l == 0:
                    S.dma("sp", dbg["d_ut"][:, :], UTd[:, :], reads=[b_UTd], writes=[b_dbg], append=True)
                    S.dma("sp", dbg["d_qt"].ap().rearrange("(c p) t -> p c t", p=128), QT[:, :, :], reads=[b_QT],
                          writes=[b_dbg], append=True)
                    S.dma("sp", dbg["d_iqt"].ap().rearrange("(c p) t -> p c t", p=128), IQT[:, :, :], reads=[b_IQT],
                          writes=[b_dbg], append=True)
                    S.dma("sp", dbg["d_aw"][:, :], AW[:, :, :].rearrange("p j h -> p (j h)"), reads=[b_AW],
                          writes=[b_dbg], append=True)
                    S.dma("sp", dbg["d_sg"][:, :], SG[:, :, :].rearrange("p j h -> p (j h)"), reads=[b_SG],
                          writes=[b_dbg], append=True)
                S.mute = CUT < 8
                for kk in range(4):
                    S.collective(xsp[kk].ap().opt(), XGp[l][kk].ap().opt(), reads=[b_XSd[l]], writes=[b_XGd[l]])
                S.mute = False
                S.barrier()
                S.flush(block)

            if stop_after == f"A{l}":
                ab.close()
                return nc
            with ExitStack() as st, nc.Block() as block:
                KT = sb(st, "KT", [128, 4, T], BF16)
                VA = sb(st, "VA", [128, 32, 512], BF16)
                IKT = sb(st, "IKT", [128, T], BF16)
                SC1 = sb(st, "SC", [128, T], F32)
                SCs = [SC1, SC1]
                MB = sb(st, "MB", [128, 2, T], BF16)
                MTs = [sb(st, f"MT{i}", [128, 32, 256], BF16) for i in range(2)]
                RR = [[sb(st, f"RR{i}{h}", [128, 512], BF16) for h in range(4)] for i in range(2)]
                PT = [sb(st, f"PT{i}", [128, 512], BF16) for i in range(4)]
                DGs = [sb(st, f"DG{i}", [128, 4, 128], BF16) for i in range(2)]
                CBd = sb(st, "CBd", [128, 8], F32)
                CBa = sb(st, "CBa", [128, 4], F32)
                CBf = sb(st, "CBf", [128, 2], F32)
                RC = [sb(st, f"RC{i}", [128, 512], F32) for i in range(2)]
                QBD = [sb(st, f"QBD{i}", [128, 4, 512], BF16) for i in range(2)]
                b_QBD = [Buf("QBD0"), Buf("QBD1")]
                for i_ in range(2):
                    S.op("pool", OP("memset", QBD[i_][:, :, :], 0.0), writes=[b_QBD[i_]])
                b_KT, b_VA, b_IKT, b_MTs = Buf("KT"), Buf("VA"), Buf("IKT"), [Buf("MT0"), Buf("MT1")]
                b_sc1 = Buf("SC")
                b_SC, b_MB, b_DG = [b_sc1, b_sc1], [Buf("MB0"), Buf("MB1")], [Buf("DG0"), Buf("DG1")]
                b_JKd, b_JKa = b_MB[0], b_MB[1]
                b_CBd2 = [Buf("CBd0"), Buf("CBd1")]
                b_RR = [[Buf(f"RR{i}{h}") for h in range(4)] for i in range(2)]
                b_PT, b_CBd, b_CBa, b_CBf, b_RC = [Buf("PT0"), Buf("PT1"), Buf("PT2"), Buf("PT3")], Buf("CBd"), Buf("CBa"), \
                    [Buf("CBf0"), Buf("CBf1")], [Buf("RC0"), Buf("RC1")]
                xgp = XGp[l]
                for pp in range(2):
                    for hp in range(4):
                        kk = hp // 2
                        r0 = pp * PR[kk] + (hp % 2) * 128
                        S.dma("sp", KT[:, hp, :].rearrange("p (j q i) -> p j q i", q=2, i=128)[:, :, pp, :],
                              xgp[kk][r0:r0 + 128, :].rearrange("p (j i) -> p j i", i=128),
                              reads=[b_XGd[l]], writes=[b_KT], append=True)
                    for r0 in (0, 64):
                        S.dma("sp", IKT[r0:r0 + 64, :].rearrange("p (j q i) -> p j q i", q=2, i=128)[:, :, pp, :],
                              xgp[1][pp * 320 + 256:pp * 320 + 320, :].rearrange("p (j i) -> p j i", i=128),
                              reads=[b_XGd[l]], writes=[b_IKT], append=True)
                for j in range(NT):
                    for pp in range(2):
                        kk = 2 + j // 8
                        vview = xgp[kk][pp * PR[kk]:pp * PR[kk] + 256, :].rearrange("r (q c) -> (r q) c", q=4)
                        jj = j % 8
                        S.dma("sp", VA[:, 2 * j + pp, :], vview[jj * 128:(jj + 1) * 128, :], reads=[b_XGd[l]],
                              writes=[b_VA], append=True)
                lrot = [0]

                def ib_units(g):
                    nch = g + 1
                    N = 512 * nch
                    nkb = 4 * nch
                    MT, b_MT = MTs[g % 2], b_MTs[g % 2]
                    U = []
                    wK = BIS_R / (2.0 ** BIS_ITERS)
                    for tau in range(2):
                        j = 2 * g + tau
                        SC, b_sc, DG, b_dg = SC1, b_sc1, DGs[tau], b_DG[tau]

                        def dg_unit(j=j, DG=DG, b_dg=b_dg):
                            for h in range(4):
                                S.op("pool", OP("tensor_scalar", DG[:, h, :], IDB[:, :], SG[:, j, h:h + 1], 1.0,
                                               ALU.mult, ALU.mult), reads=[b_SG, b_const], writes=[b_dg])
                        U.append(dg_unit)
                        for c in range(nch):
                            ri = c % 2
                            for h in range(4):
                                def lg_unit(c=c, j=j, h=h, ri=ri):
                                    pr = (h % 2) * 64
                                    bk = 6 + lrot[0] % 2
                                    lrot[0] += 1
                                    S.op("pe", OP("matmul", PS[bk][:, :], IQT[pr:pr + 64, h // 2, j * 128:(j + 1) * 128],
                                                  IKT[pr:pr + 64, c * 512:(c + 1) * 512], start=True, stop=True),
                                         reads=[b_IQT, b_IKT], writes=[b_PS[bk]])
                                    S.op("act", OP("activation", RR[ri][h][:, :], PS[bk][:, :], AF.Relu,
                                                   scale=AW[:, j, h:h + 1]),
                                         reads=[b_PS[bk], b_AW], writes=[b_RR[ri][h]])
                                U.append(lg_unit)

                            def cmb_unit(c=c, tau=tau, DG=DG, b_dg=b_dg, ri=ri):
                                bk = 6 + lrot[0] % 2
                                lrot[0] += 1
                                for h in range(4):
                                    S.op("pe", OP("matmul", PS[bk][:, :], DG[:, h, :], RR[ri][h][:, :],
                                                  start=(h == 0), stop=(h == 3)),
                                         reads=[b_dg, b_RR[ri][h]], writes=[b_PS[bk]], sig=(h == 3))
                                if c == nch - 1:
                                    S.op("dve", OP("tensor_tensor", SC1[:, c * 512:(c + 1) * 512], PS[bk][:, :],
                                                   ADM[:, tau, :], ALU.add),
                                         reads=[b_PS[bk], b_const], writes=[b_sc1])
                                else:
                                    S.op("act", OP("activation", SC1[:, c * 512:(c + 1) * 512], PS[bk][:, :], AF.Copy),
                                         reads=[b_PS[bk]], writes=[b_sc1])
                            U.append(cmb_unit)
                        if j == 0:
                            U.append(lambda: S.op("dve", OP("memset", CBf[:, 0:1], -10000.0), writes=[b_CBf[0]]))
                        else:
                            U.append(lambda: S.op("dve", OP("memset", CBd[:, 0:1], 0.0), writes=[b_CBd]))
                            for k in range(1, BIS_ITERS + 1):
                                def bis_d(k=k):
                                    wk = BIS_R / (2.0 ** k)
                                    S.op("dve", OP("tensor_scalar", MB[:, 1, 0:N], SC1[:, 0:N], CBd[:, 0:1], None,
                                                   ALU.is_ge, ALU.add, accum_out=CBd[:, 1:2]),
                                         reads=[b_sc1, b_CBd], writes=[b_MB[1], b_CBd])
                                    S.op("dve", OP("tensor_scalar", CBd[:, 2:3], CBd[:, 1:2], 255.5, -0.5,
                                                   ALU.is_ge, ALU.add), reads=[b_CBd], writes=[b_CBd])
                                    S.op("dve", OP("scalar_tensor_tensor", CBd[:, 0:1], CBd[:, 2:3], 2.0 * wk,
                                                   CBd[:, 0:1], ALU.mult, ALU.add), reads=[b_CBd], writes=[b_CBd])
                                U.append(bis_d)
                            U.append(lambda tau=tau: S.op(
                                "dve", OP("tensor_scalar", CBf[:, tau:tau + 1], CBd[:, 0:1], -wK, None, ALU.add),
                                reads=[b_CBd], writes=[b_CBf[tau]]))
                        U.append(lambda tau=tau: S.op(
                            "dve", OP("tensor_scalar", MB[:, tau, 0:N], SC1[:, 0:N], CBf[:, tau:tau + 1], None,
                                      ALU.is_ge), reads=[b_sc1, b_CBf[tau]], writes=[b_MB[tau]]))
                    for k2 in range(nkb // 2):
                        def tr_unit(k2=k2):
                            bk = 6 + lrot[0] % 2
                            lrot[0] += 1
                            for ee in range(2):
                                kb = 2 * k2 + ee
                                for tau in range(2):
                                    S.op("pe", OP("matmul", PS[bk][:, ee * 256 + tau * 128:ee * 256 + (tau + 1) * 128],
                                                  MB[:, tau, kb * 128:(kb + 1) * 128], IDB[:, :], start=True,
                                                  stop=True),
                                         reads=[b_MB[tau], b_const], writes=[b_PS[bk]], sig=(ee == 1 and tau == 1))
                            S.op("act", OP("activation", MT[:, 2 * k2:2 * k2 + 2, :].rearrange("p a t -> p (a t)"),
                                           PS[bk][:, :], AF.Copy), reads=[b_PS[bk]], writes=[b_MT])
                        U.append(tr_unit)
                    return U

                def qbd_build(g):
                    t0 = 2 * g * 128
                    qb, bq = QBD[g % 2], b_QBD[g % 2]
                    for hp in range(4):
                        S.op("act", OP("activation", qb[0:64, hp, 0:256], QT[0:64, hp, t0:t0 + 256], AF.Copy),
                             reads=[b_QT], writes=[bq])
                        S.op("act", OP("activation", qb[64:128, hp, 256:512], QT[64:128, hp, t0:t0 + 256], AF.Copy),
                             reads=[b_QT], writes=[bq])

                def att_units(g):
                    nkb = 4 * (g + 1)
                    MT, b_MT = MTs[g % 2], b_MTs[g % 2]
                    qb, bq = QBD[g % 2], b_QBD[g % 2]
                    units = [(hp, kb) for hp in range(4) for kb in range(nkb)]

                    def stage12(i):
                        hp, kb = units[i]
                        bs, pi = i % 2, i % 4
                        S.op("pe", OP("matmul", PS[bs][:, :], KT[:, hp, kb * 128:(kb + 1) * 128], qb[:, hp, :],
                                      start=True, stop=True),
                             reads=[b_KT, bq], writes=[b_PS[bs]])
                        S.op("act", OP("activation", PT[pi][:, :], PS[bs][:, :], AF.Exp, scale=0.125),
                             reads=[b_PS[bs]], writes=[b_PT[pi]])
                        S.op("pool", OP("tensor_tensor", PT[pi][:, :].rearrange("p (a t) -> p a t", a=2),
                                        PT[pi][:, :].rearrange("p (a t) -> p a t", a=2),
                                        MT[:, kb:kb + 1, :].to_broadcast([128, 2, 256]), ALU.mult),
                             reads=[b_PT[pi], b_MT], writes=[b_PT[pi]])

                    def stage3(i):
                        hp, kb = units[i]
                        pi = i % 4
                        bo, br = 2 + hp % 2, 4 + hp % 2
                        S.op("pe", OP("matmul", PS[bo][:, :], VA[:, kb, hp * 128:(hp + 1) * 128], PT[pi][:, :],
                                      start=(kb == 0), stop=(kb == nkb - 1)),
                             reads=[b_VA, b_PT[pi]], writes=[b_PS[bo]], sig=(kb == nkb - 1))
                        S.op("pe", OP("matmul", PS[br][:, :], ONES[:, :], PT[pi][:, :],
                                      start=(kb == 0), stop=(kb == nkb - 1)),
                             reads=[b_const, b_PT[pi]], writes=[b_PS[br]])
                    return units, stage12, stage3

                def att_norm(g, hp):
                    t0 = 2 * g * 128
                    bo, br = 2 + hp % 2, 4 + hp % 2
                    ri = hp % 2
                    for e2 in range(2):
                        pr, c0 = e2 * 64, e2 * 256
                        S.op("dve", OP("reciprocal", RC[ri][pr:pr + 64, c0:c0 + 256], PS[br][pr:pr + 64, c0:c0 + 256]),
                             reads=[b_PS[br]], writes=[b_RC[ri]])
                        S.op("dve", OP("tensor_tensor", AT[pr:pr + 64, hp, t0:t0 + 256], PS[bo][pr:pr + 64, c0:c0 + 256],
                                       RC[ri][pr:pr + 64, c0:c0 + 256], ALU.mult),
                             reads=[b_PS[bo], b_RC[ri]], writes=[b_AT])

                for u in ib_units(0):
                    u()
                qbd_build(0)
                for g in range(8):
                    ibu = ib_units(g + 1) if g < 7 else []
                    n = len(ibu)
                    units, stage12, stage3 = att_units(g)
                    U = len(units)
                    per_h = U // 4
                    ib_done = 0
                    SK = 2
                    for i in range(U + SK):
                        if i < U:
                            stage12(i)
                        if i >= SK:
                            i3 = i - SK
                            stage3(i3)
                            tgt = min(n, ((i3 + 1) * n * 5 + 3 * U - 1) // (3 * U))
                            for u in ibu[ib_done:tgt]:
                                u()
                            ib_done = tgt
                            hprev, kbprev = units[i3]
                            if kbprev == per_h - 1:
                                att_norm(g, hprev)
                            if conv_th and (i3 % 6 == 0):
                                conv_th.pop(0)()
                        if i == U // 2 and g < 7:
                            qbd_build(g + 1)
                    for u in ibu[ib_done:]:
                        u()
                while conv_th:
                    conv_th.pop(0)()
                if debug and l == 0:
                    S.dma("sp", dbg["d_at"].ap().rearrange("(c p) t -> p c t", p=128), AT[:, :, :], reads=[b_AT],
                          writes=[b_dbg], append=True)
                S.barrier()
                S.flush(block)

            ab.close()
            if stop_after == f"B{l}":
                return nc
            with ExitStack() as st, nc.Block() as block:
                XT = sb(st, "XTc", [128, 4, D], F32)
                GF = sb(st, "GF", [128, D], F32)
                b_GF = Buf("GF")
                if last:
                    S.dma("sp", GF[:, :], n_fin.ap().partition_broadcast(128), writes=[b_GF])
                UTgL = [sb(st, f"UTc{i}", [128, 8, 512], BF16) for i in range(2)]
                U2 = sb(st, "U2", [128, 8, 512], BF16)
                CHBL = [sb(st, f"CHB{i}", [128, 4, 4, 130], BF16) for i in range(2)]
                HAL = [sb(st, f"HA{i}", [128, 4, 4, 2], BF16) for i in range(2)]
                HBL = [sb(st, f"HB{i}", [128, 4, 4, 2], BF16) for i in range(2)]
                b_UTgL, b_CHBL = [Buf("UTc0"), Buf("UTc1")], [Buf("CHB0"), Buf("CHB1")]
                b_HAL, b_HBL = [Buf("HA0"), Buf("HA1")], [Buf("HB0"), Buf("HB1")]
                HT1 = sb(st, "HT1", [128, 4, 4, 2], F32)
                CV = sb(st, "CV", [128, 4, 128], F32)
                ZT = sb(st, "ZT", [128, 4, 512], BF16)
                SG0 = sb(st, "SG0", [128, 512], F32)
                SG1 = sb(st, "SG1", [128, 512], F32)
                M1 = sb(st, "M1", [128, 512], F32)
                M2 = sb(st, "M2", [128, 512], F32)
                MG = sb(st, "MG", [128, 8, 512], BF16)
                HT = sb(st, "HT", [128, 32, 512], BF16)
                RL = [sb(st, f"RL{i}", [128, 512], F32) for i in range(2)]
                XS2 = [sb(st, f"XS2c{i}", [128, D], F32) for i in range(2)]
                SSQ = sb(st, "SSQc", [128, 16], F32)
                b_x2c, b_ssc = [Buf("x2c0"), Buf("x2c1")], Buf("ssc")
                b_XT, b_UTg, b_U2, b_CHB, b_HA, b_HB, b_HT1, b_CV, b_ZT = (Buf(n) for n in (
                    "XTc", "UTc", "U2", "CHB", "HA", "HB", "HT1", "CV", "ZT"))
                b_SG0, b_SG1, b_M1, b_M2, b_MG, b_HT, b_nt, b_OT = (Buf(n) for n in (
                    "SG0", "SG1", "M1", "M2", "MG", "HT", "ntc", "OT"))
                b_RL = [Buf("RL0"), Buf("RL1")]
                loads = []
                for G in range(4):
                    loads += [(wsrc(WinB[l], C_CB, 512), 8, 512, b_W[("in", l)])]
                    for fh in range(2):
                        loads += [(wsrc(WaoB[l], fh * 512, 512, 0, 4), 4, 512, b_W[("ao", l)]),
                                  (wsrc(WcoB[l], fh * 512, 512, 0, 4), 4, 512, b_W[("co", l)]),
                                  (wsrc(WinB[l], C_G + fh * 512, 512), 8, 512, b_W[("in", l)]),
                                  (wsrc(WinB[l], C_G + 1024 + fh * 512, 512), 8, 512, b_W[("in", l)])]
                    loads += [(wsrc(WmxB[l], hf * 512, 512), 8, 512, b_W[("mx", l)]) for hf in range(2)]
                    loads += [(wsrc(WupB[l], i * 512, 512), 8, 512, b_W[("up", l)]) for i in range(8)]
                    loads += [(wsrc(WdnB[l], hf * 512, 512, hg * 8, 8), 8, 512, b_W[("dn", l)])
                              for hf in range(2) for hg in range(4)]
                NLG = len(loads) // 4
                ws = WStream(st, 7, loads, 4)
                xgp = XGp[l]
                chv = CHd.ap().rearrange("(c p) (j w) -> p c j w", p=128, w=130)

                def halo_view(pp):
                    base = pp * 264 + 256
                    return xgp[3][base:base + 8, :].rearrange("r (a b) -> (r a) b", b=32).rearrange(
                        "(c p) (j w) -> p c j w", p=128, w=2)

                def prefetch(G):
                    g0_ = G * 512
                    k_ = G % 2
                    S.dma("sp", UTgL[k_][:, :, :], UTd.ap().rearrange("(k p) t -> p k t", p=128)[:, :, g0_:g0_ + 512],
                          reads=[b_UTd], writes=[b_UTgL[k_]])
                    for c in range(4):
                        S.dma("sp", CHBL[k_][:, c, :, 2:130], chv[:, c, G * 4:G * 4 + 4, 2:130], reads=[b_CHd],
                              writes=[b_CHBL[k_]], append=(c > 0))
                    for c in range(4):
                        S.dma("sp", HAL[k_][:, c, :, :], halo_view(0)[:, c, G * 4:G * 4 + 4, :], reads=[b_XGd[l]],
                              writes=[b_HAL[k_]], append=(c > 0))
                    S.op("pool", OP("memset", HBL[k_][:, :, :, :], 0.0), writes=[b_HBL[k_]])
                    for c in range(4):
                        if G == 0:
                            S.dma("sp", HBL[k_][:, c, 1:4, :], halo_view(1)[:, c, 0:3, :], reads=[b_XGd[l]],
                                  writes=[b_HBL[k_]], append=True)
                        else:
                            S.dma("sp", HBL[k_][:, c, :, :], halo_view(1)[:, c, G * 4 - 1:G * 4 + 3, :],
                                  reads=[b_XGd[l]], writes=[b_HBL[k_]], append=True)

                prefetch(0)
                for G in range(4):
                    g0 = G * 512
                    wi = G * NLG
                    UTg, b_UTg, CHB, b_CHB = UTgL[G % 2], b_UTgL[G % 2], CHBL[G % 2], b_CHBL[G % 2]
                    HA, b_HA, HB, b_HB = HAL[G % 2], b_HAL[G % 2], HBL[G % 2], b_HBL[G % 2]
                    S.dma("sp", XT[:, :, :], x_src[g0:g0 + 512, :].rearrange("(t p) d -> p t d", p=128),
                          reads=[b_xsrc], writes=[b_XT])
                    S.op("dve", OP("tensor_scalar", HT1[:, :, :, :], HA[:, :, :, :], SEL[:, 0:1], None, ALU.mult),
                         reads=[b_HA, b_const], writes=[b_HT1])
                    S.op("dve", OP("scalar_tensor_tensor",
                        CHB[:, :, :, 0:2].rearrange("p c j w -> p (c j) w"),
                        HB[:, :, :, :].rearrange("p c j w -> p (c j) w"), SEL[:, 1:2],
                        HT1[:, :, :, :].rearrange("p c j w -> p (c j) w"), ALU.mult, ALU.add),
                         reads=[b_HB, b_HT1, b_const, b_CHB], writes=[b_CHB])
                    Wt, b_w = ws.get(wi + 0)
                    for c in range(4):
                        bk = c % 2
                        for kc in range(8):
                            S.op("pe", OP("matmul",
                                PS[bk][:, :], Wt[:, kc, c * 128:(c + 1) * 128], UTg[:, kc, :], start=(kc == 0),
                                stop=(kc == 7)), reads=[b_w, b_UTg], writes=[b_PS[bk]], sig=(kc == 7))
                        S.op("dve", OP("tensor_scalar", CV[:, :, :], CHB[:, c, :, 2:130], CW[:, l, c, 2:3],
                                                                   None, ALU.mult),
                             reads=[b_CHB, b_const], writes=[b_CV])
                        for jt in (1, 0):
                            S.op("dve", OP("scalar_tensor_tensor",
                                CV[:, :, :], CHB[:, c, :, jt:jt + 128], CW[:, l, c, jt:jt + 1], CV[:, :, :], ALU.mult,
                                ALU.add), reads=[b_CHB, b_const, b_CV], writes=[b_CV])
                        S.op("dve", OP("tensor_tensor",
                            ZT[:, c, :], PS[bk][:, :], CV[:, :, :].rearrange("p t w -> p (t w)"), ALU.mult),
                             reads=[b_PS[bk], b_CV], writes=[b_ZT])
                    for f in range(8):
                        if f % 4 == 0:
                            fh = f // 4
                            Wa, b_wa = ws.get(wi + 1 + 4 * fh)
                            Wc, b_wc = ws.get(wi + 2 + 4 * fh)
                            Wg0, b_wg0 = ws.get(wi + 3 + 4 * fh)
                            Wg1, b_wg1 = ws.get(wi + 4 + 4 * fh)
                        fo = (f % 4) * 128
                        ba, bc_, bg0, bg1 = (0, 1, 2, 3) if f % 2 == 0 else (4, 5, 6, 7)
                        for kc in range(4):
                            S.op("pe", OP("matmul",
                                PS[ba][:, :], Wa[:, kc, fo:fo + 128], AT[:, kc, g0:g0 + 512],
                                start=(kc == 0), stop=(kc == 3)), reads=[b_wa, b_AT], writes=[b_PS[ba]],
                                 sig=(kc == 3))
                        for kc in range(4):
                            S.op("pe", OP("matmul",
                                PS[bc_][:, :], Wc[:, kc, fo:fo + 128], ZT[:, kc, :],
                                start=(kc == 0), stop=(kc == 3)), reads=[b_wc, b_ZT], writes=[b_PS[bc_]],
                                 sig=(kc == 3))
                        for (Wg, b_wg, bg) in ((Wg0, b_wg0, bg0), (Wg1, b_wg1, bg1)):
                            for kc in range(8):
                                S.op("pe", OP("matmul",
                                    PS[bg][:, :], Wg[:, kc, fo:fo + 128], UTg[:, kc, :], start=(kc == 0),
                                    stop=(kc == 7)), reads=[b_wg, b_UTg], writes=[b_PS[bg]], sig=(kc == 7))
                        S.op("act", OP("activation", SG0[:, :], PS[bg0][:, :], AF.Sigmoid),
                             reads=[b_PS[bg0]], writes=[b_SG0])
                        S.op("act", OP("activation", SG1[:, :], PS[bg1][:, :], AF.Sigmoid),
                             reads=[b_PS[bg1]], writes=[b_SG1])
                        S.op("dve", OP("tensor_tensor", M1[:, :], PS[ba][:, :], SG0[:, :], ALU.mult),
                             reads=[b_PS[ba], b_SG0], writes=[b_M1])
                        S.op("dve", OP("tensor_tensor", M2[:, :], PS[bc_][:, :], SG1[:, :], ALU.mult),
                             reads=[b_PS[bc_], b_SG1], writes=[b_M2])
                        S.op("pool", OP("tensor_tensor", MG[:, f, :], M1[:, :], M2[:, :], ALU.add),
                             reads=[b_M1, b_M2], writes=[b_MG])
                    for hf in range(2):
                        Wm, b_wm = ws.get(wi + 9 + hf)
                        for tau in range(4):
                            bk = (hf * 4 + tau) % 4
                            for kc in range(8):
                                S.op("pe", OP("matmul",
                                    PS[bk][:, :], MG[:, kc, tau * 128:(tau + 1) * 128], Wm[:, kc, :],
                                    start=(kc == 0), stop=(kc == 7)), reads=[b_wm, b_MG], writes=[b_PS[bk]],
                                     sig=(kc == 7))
                            S.op("dve", OP("tensor_tensor",
                                XT[:, tau, hf * 512:(hf + 1) * 512], PS[bk][:, :], XT[:, tau, hf * 512:(hf + 1) * 512],
                                ALU.add), reads=[b_PS[bk], b_XT], writes=[b_XT])
                    if G < 3:
                        prefetch(G + 1)
                    norm_group((XS2, SSQ, b_x2c, b_ssc), XT, b_XT, GP[:, l, :], U2, b_U2)
                    for hc in range(32):
                        if hc % 4 == 0:
                            Wu, b_wu = ws.get(wi + 11 + hc // 4)
                        bk = hc % 4
                        ho = (hc % 4) * 128
                        for kc in range(8):
                            S.op("pe", OP("matmul",
                                PS[bk][:, :], Wu[:, kc, ho:ho + 128], U2[:, kc, :], start=(kc == 0), stop=(kc == 7)),
                                 reads=[b_wu, b_U2], writes=[b_PS[bk]], sig=(kc == 7))
                        S.op("act", OP("activation", RL[hc % 2][:, :], PS[bk][:, :], AF.Relu),
                             reads=[b_PS[bk]], writes=[b_RL[hc % 2]])
                        S.op("pool", OP("tensor_tensor", HT[:, hc, :], RL[hc % 2][:, :], RL[hc % 2][:, :],
                                                                      ALU.mult),
                             reads=[b_RL[hc % 2]], writes=[b_HT])
                    for hf in range(2):
                        for hg in range(4):
                            Wd, b_wd = ws.get(wi + 19 + hf * 4 + hg)
                            for tau in range(4):
                                bk = 4 + tau
                                for k8 in range(8):
                                    hc = hg * 8 + k8
                                    S.op("pe", OP("matmul",
                                        PS[bk][:, :], HT[:, hc, tau * 128:(tau + 1) * 128], Wd[:, k8, :],
                                        start=(hc == 0), stop=(hc == 31)), reads=[b_wd, b_HT], writes=[b_PS[bk]],
                                         sig=(k8 == 7))
                        for tau in range(4):
                            bk = 4 + tau
                            S.op("dve", OP("tensor_tensor",
                                XT[:, tau, hf * 512:(hf + 1) * 512], PS[bk][:, :], XT[:, tau, hf * 512:(hf + 1) * 512],
                                ALU.add), reads=[b_PS[bk], b_XT], writes=[b_XT])
                    if not last:
                        S.dma("sp", XRd[g0:g0 + 512, :].rearrange("(t p) d -> p t d", p=128), XT[:, :, :],
                              reads=[b_XT], writes=[b_XRd], append=True)
                    else:
                        for tau in range(4):
                            S.op("act", OP("activation", XS2[tau % 2][:, :], XT[:, tau, :], AF.Square,
                                           accum_out=SSQ[:, tau:tau + 1]), reads=[b_XT],
                                 writes=[b_x2c[tau % 2], b_ssc])
                        S.op("dve", OP("tensor_scalar", SSQ[:, 4:8], SSQ[:, 0:4], 1.0 / D, EPS, ALU.mult, ALU.add),
                             reads=[b_ssc], writes=[b_ssc])
                        S.op("act", OP("activation", SSQ[:, 8:12], SSQ[:, 4:8], AF.Sqrt), reads=[b_ssc],
                             writes=[b_ssc])
                        S.op("dve", OP("reciprocal", SSQ[:, 12:16], SSQ[:, 8:12]), reads=[b_ssc], writes=[b_ssc])
                        for tau in range(4):
                            ot, bo_ = XS2[tau % 2], b_x2c[tau % 2]
                            S.op("dve", OP("scalar_tensor_tensor", ot[:, :], XT[:, tau, :], SSQ[:, 12 + tau:13 + tau],
                                           GF[:, :], ALU.mult, ALU.mult),
                                 reads=[b_XT, b_ssc, b_GF], writes=[bo_])
                            S.dma("sp", out_d[g0 + tau * 128:g0 + (tau + 1) * 128, :], ot[:, :], reads=[bo_],
                                  writes=[b_out], append=True)
                S.barrier()
                S.flush(block)
    return nc


_CACHE = {}


def _rope_tables():
    inv = (1.0 / (np.float32(500000.0) ** (np.arange(0, 16, 2, dtype=np.float32) / np.float32(16)))).astype(np.float32)
    ang = (np.arange(T, dtype=np.float32)[:, None] * inv[None, :]).astype(np.float32)
    return np.cos(ang).astype(np.float32), np.sin(ang).astype(np.float32)


def _core_inputs(inputs, debug=False):
    cos, sin = _rope_tables()
    ident = np.eye(128, dtype=np.float32)
    shared = {k: np.ascontiguousarray(np.asarray(inputs[k], dtype=np.float32)) for k in
              ("w_in", "w_attn_out", "w_conv_out", "w_mix_out", "w_mlp_up", "w_mlp_down", "norm_mix", "norm_mlp",
               "norm_final", "conv_w")}
    x = np.asarray(inputs["x"], dtype=np.float32)
    maps = []
    for core in range(NCORES):
        b, p = core // 2, core % 2
        tiles = np.arange(NT) * 2 + p
        tok = (tiles[:, None] * 128 + np.arange(128)[None, :]).reshape(-1)
        xc = np.ascontiguousarray(x[b][tok])
        c = cos[tok].T
        s = sin[tok].T
        ropeC = np.ascontiguousarray(np.concatenate([c, c], 0))
        ropeS = np.ascontiguousarray(np.concatenate([-s, s], 0))
        adm = np.zeros((128, 2, 512), np.float32)
        col = np.arange(512)[None, :]
        tt = np.arange(128)[:, None]
        for tau in range(2):
            lim = 128 * (2 * tau + p) + 64 + 64 * (tt >= 64)
            adm[:, tau, :] = np.where(col < lim, 0.0, NEG)
        sel = np.zeros((128, 2), np.float32)
        sel[:, 0] = 1.0 if p == 1 else 0.0
        sel[:, 1] = 1.0 if p == 0 else 0.0
        m = dict(shared)
        m.update({"x": xc, "ropeC": ropeC, "ropeS": ropeS, "adm": adm, "sel": sel, "ident": ident})
        maps.append(m)
    return maps


def kernel(**inputs):
    if "nc" not in _CACHE:
        _CACHE["nc"] = build_program()
    nc = _CACHE["nc"]
    maps = _core_inputs(inputs)
    res = run_bass_kernel_spmd(nc, maps, core_ids=list(range(NCORES)))
    out = np.empty((NB, T, D), np.float32)
    for core in range(NCORES):
        b, p = core // 2, core % 2
        o = np.asarray(res.results[core]["out"], dtype=np.float32).reshape(NT, 128, D)
        for j in range(NT):
            m = 2 * j + p
            out[b, m * 128:(m + 1) * 128, :] = o[j]
    return out
```

```python
import os
import numpy as np
from contextlib import ExitStack
import concourse.bass as bass
import concourse.mybir as mybir
from concourse.bass_utils import run_bass_kernel_spmd

F32 = mybir.dt.float32
BF16 = mybir.dt.bfloat16
AF = mybir.ActivationFunctionType
ALU = mybir.AluOpType

D = 1024
T = 4096
NB = 4
DEPTH = 2
NCORES = 8
TL = 2048
NT = 16
INC = 5444
C_Q, C_K, C_V, C_IQ, C_IK, C_IW, C_CB, C_CC, C_CH, C_G = 0, 512, 1024, 1536, 1792, 1856, 1860, 2372, 2884, 3396
HID = 4096
EPS = 1e-6
XS_ROWS = 1096
NEG = -30000.0
BIS_ITERS = 10
BIS_R = 4.0
ACT_BISECT = False
SAME_ENG_SYNC = True
NDSEM = 32
NCONV = 48
CUT = int(os.environ.get('K_CUT', '99'))


def OP(name, *a, **k):
    return (name, a, k)


class Buf:
    __slots__ = ("name", "w", "r")

    def __init__(self, name):
        self.name = name
        self.w = []
        self.r = {}


class Sched:
    def __init__(self, nc, sems, dsems):
        self.nc = nc
        self.q = {k: [] for k in ("pe", "act", "dve", "pool", "sp")}
        self.cnt = {k: 0 for k in self.q}
        self.cnt["cc"] = 0
        self.known = {k: {} for k in self.q}
        self.sems = sems
        self.dsems = dsems
        self.dcnt = [0] * len(dsems)
        self.dnext = 0
        self.mute = False

    def _wait(self, eng, tok):
        kind, key, val = tok
        if kind == "e" and key == eng and (eng == "pe" or not SAME_ENG_SYNC):
            return
        k = (kind, key)
        if self.known[eng].get(k, 0) >= val:
            return
        self.known[eng][k] = val
        sem = self.sems[key] if kind == "e" else self.dsems[key]
        self.q[eng].append(OP("wait_ge", sem, val))

    def _deps(self, eng, reads, writes):
        for b in reads:
            for t in b.w:
                self._wait(eng, t)
        for b in writes:
            for t in b.w:
                self._wait(eng, t)
            for t in b.r.values():
                self._wait(eng, t)

    def _mark(self, tok, reads, writes, append=False):
        for b in reads:
            b.r[(tok[0], tok[1])] = tok
        for b in writes:
            if append:
                b.w.append(tok)
            else:
                b.w = [tok]
                b.r = {}

    def op(self, eng, fn, reads=(), writes=(), sig=True):
        if self.mute:
            return
        self._deps(eng, reads, writes)
        if sig:
            self.cnt[eng] += 1
            tok = ("e", eng, self.cnt[eng])
            sem = self.sems[eng]
            self.q[eng].append(lambda e, fn=fn, sem=sem: getattr(e, fn[0])(*fn[1], **fn[2]).then_inc(sem, 1))
        else:
            tok = ("e", eng, self.cnt[eng] + 1)
            self.q[eng].append(lambda e, fn=fn: getattr(e, fn[0])(*fn[1], **fn[2]))
        self._mark(tok, reads, writes)

    def dma(self, qeng, out_ap, in_ap, reads=(), writes=(), append=False, fixed=None, **kw):
        if self.mute:
            return
        self._deps(qeng, reads, writes)
        if fixed is not None:
            i = NDSEM + fixed
        else:
            i = self.dnext
            self.dnext = (i + 1) % NDSEM
        if self.dcnt[i] > 0:
            self._wait(qeng, ("d", i, 16 * self.dcnt[i]))
        self.dcnt[i] += 1
        tok = ("d", i, 16 * self.dcnt[i])
        sem = self.dsems[i]
        self.q[qeng].append(
            lambda e, o=out_ap, a=in_ap, sem=sem, kw=kw: e.dma_start(out=o, in_=a, **kw).then_inc(sem, 16))
        self._mark(tok, reads, writes, append=append)

    def collective(self, ins_ap, outs_ap, reads, writes):
        if self.mute:
            return
        eng = "pool"
        self._deps(eng, reads, writes)
        self.cnt["cc"] += 1
        tok = ("e", "cc", self.cnt["cc"])
        sem = self.sems["cc"]
        groups = [[0, 1], [2, 3], [4, 5], [6, 7]]
        self.q[eng].append(
            lambda e, a=ins_ap, o=outs_ap, sem=sem: e.collective_compute(
                "AllGather", ALU.bypass, replica_groups=groups, ins=[a], outs=[o]).then_inc(sem))
        self._mark(tok, reads, writes)

    def barrier(self):
        toks = [("e", k, self.cnt[k]) for k in ("pe", "act", "dve", "pool", "sp", "cc") if self.cnt[k] > 0]
        toks += [("d", i, 16 * c) for i, c in enumerate(self.dcnt) if c > 0]
        for eng in self.q:
            for t in toks:
                if t[0] == "e" and t[1] == eng:
                    continue
                self._wait(eng, t)

    def flush(self, block):
        q = self.q

        def run(lst, e):
            for f in lst:
                if isinstance(f, tuple):
                    getattr(e, f[0])(*f[1], **f[2])
                else:
                    f(e)

        @block.tensor
        def _(e):
            run(q["pe"], e)

        @block.scalar
        def _(e):
            run(q["act"], e)

        @block.vector
        def _(e):
            run(q["dve"], e)

        @block.gpsimd
        def _(e):
            run(q["pool"], e)

        @block.sync
        def _(e):
            run(q["sp"], e)

        self.q = {k: [] for k in q}


def build_program(debug=False, nlayers=DEPTH, stop_after=None):
    nc = bass.Bass("TRN2", target_bir_lowering=False)
    dt = nc.dram_tensor
    x_in = dt("x", [TL, D], F32, kind="ExternalInput")
    w_in = dt("w_in", [DEPTH, D, INC], F32, kind="ExternalInput")
    w_ao = dt("w_attn_out", [DEPTH, 512, D], F32, kind="ExternalInput")
    w_co = dt("w_conv_out", [DEPTH, 512, D], F32, kind="ExternalInput")
    w_mx = dt("w_mix_out", [DEPTH, D, D], F32, kind="ExternalInput")
    w_up = dt("w_mlp_up", [DEPTH, D, HID], F32, kind="ExternalInput")
    w_dn = dt("w_mlp_down", [DEPTH, HID, D], F32, kind="ExternalInput")
    n_mix = dt("norm_mix", [DEPTH, D], F32, kind="ExternalInput")
    n_mlp = dt("norm_mlp", [DEPTH, D], F32, kind="ExternalInput")
    n_fin = dt("norm_final", [D], F32, kind="ExternalInput")
    conv_w = dt("conv_w", [DEPTH, 3, 512], F32, kind="ExternalInput")
    ropeC = dt("ropeC", [16, TL], F32, kind="ExternalInput")
    ropeS = dt("ropeS", [16, TL], F32, kind="ExternalInput")
    adm_in = dt("adm", [128, 2, 512], F32, kind="ExternalInput")
    sel_in = dt("sel", [128, 2], F32, kind="ExternalInput")
    ident_in = dt("ident", [128, 128], F32, kind="ExternalInput")
    out_d = dt("out", [TL, D], F32, kind="ExternalOutput")
    dbg = {}
    if debug:
        for nm, shp, ty in [("d_ut", [D, TL], BF16), ("d_xs", [XS_ROWS, TL], BF16), ("d_qt", [512, TL], BF16),
                            ("d_iqt", [256, TL], BF16), ("d_at", [512, TL], BF16), ("d_aw", [128, 64], F32),
                            ("d_sg", [128, 64], F32), ("d_sc", [128, 4096], F32), ("d_mb", [128, 8192], BF16),
                            ("d_cb", [128, 1], F32)]:
            dbg[nm] = dt(nm, shp, ty, kind="ExternalOutput")

    WinB = [dt(f"WinB{l}", [D, INC], BF16) for l in range(DEPTH)]
    WaoB = [dt(f"WaoB{l}", [512, D], BF16) for l in range(DEPTH)]
    WcoB = [dt(f"WcoB{l}", [512, D], BF16) for l in range(DEPTH)]
    WmxB = [dt(f"WmxB{l}", [D, D], BF16) for l in range(DEPTH)]
    WupB = [dt(f"WupB{l}", [D, HID], BF16) for l in range(DEPTH)]
    WdnB = [dt(f"WdnB{l}", [HID, D], BF16) for l in range(DEPTH)]
    UTd = dt("UTd", [D, TL], BF16)
    CHd = dt("CHd", [512, NT * 130], BF16)
    XRd = dt("XRd", [TL, D], F32)
    PR = [256, 320, 256, 264]
    XSp = [[dt(f"XSp{l}_{k}", [PR[k], TL], BF16) for k in range(4)] for l in range(DEPTH)]
    XGp = [[dt(f"XGp{l}_{k}", [2 * PR[k], TL], BF16) for k in range(4)] for l in range(DEPTH)]

    b_W = {(nm, l): Buf(f"{nm}{l}") for nm in ("in", "ao", "co", "mx", "up", "dn") for l in range(DEPTH)}
    b_UTd, b_CHd, b_XRd = Buf("UTd"), Buf("CHd"), Buf("XRd")
    b_XSd = [Buf("XSd0"), Buf("XSd1")]
    b_XGd = [Buf("XGd0"), Buf("XGd1")]
    b_out = Buf("out")
    b_dbg = Buf("dbg")

    with ExitStack() as top:
        sems = {k: top.enter_context(nc.semaphore(f"s_{k}")) for k in ("pe", "act", "dve", "pool", "sp", "cc")}
        dsems = [top.enter_context(nc.semaphore(f"d_{i}")) for i in range(NDSEM + NCONV)]
        S = Sched(nc, sems, dsems)

        uid = [0]

        def sb(st, name, shape, dtype):
            uid[0] += 1
            return st.enter_context(nc.sbuf_tensor(f"{name}_{uid[0]}", shape, dtype))

        AT = sb(top, "AT", [128, 4, TL], BF16)
        AW = sb(top, "AW", [128, NT, 4], F32)
        SG = sb(top, "SG", [128, NT, 4], F32)
        IDF = sb(top, "IDF", [128, 128], F32)
        IDB = sb(top, "IDB", [128, 128], BF16)
        ONES = sb(top, "ONES", [128, 128], BF16)
        ADM = sb(top, "ADM", [128, 2, 512], F32)
        SEL = sb(top, "SEL", [128, 2], F32)
        GM = sb(top, "GM", [128, DEPTH, 8], F32)
        GP = sb(top, "GP", [128, DEPTH, 8], F32)
        CW = sb(top, "CW", [128, DEPTH, 4, 3], F32)
        b_QT, b_IQT, b_AT, b_AW, b_SG = Buf("QT"), Buf("IQT"), Buf("AT"), Buf("AW"), Buf("SG")
        b_const = Buf("const")
        PS = [top.enter_context(nc.psum_tensor(f"ps{i}", [128, 512], F32)) for i in range(8)]
        b_PS = [Buf(f"ps{i}") for i in range(8)]

        with nc.Block() as block:
            S.dma("sp", IDF[:, :], ident_in[:, :], writes=[b_const], append=True)
            S.dma("pool", IDB[:, :], ident_in[:, :], writes=[b_const], append=True, fixed=40)
            S.dma("sp", ADM[:, :, :], adm_in[:, :, :], writes=[b_const], append=True)
            S.dma("sp", SEL[:, :], sel_in[:, :], writes=[b_const], append=True)
            for l in range(DEPTH):
                S.dma("sp", GM[:, l, :], n_mix[l, :].rearrange("(k p) -> p k", p=128), writes=[b_const],
                      append=True, allow_slow_non_contiguous=True)
                S.dma("sp", GP[:, l, :], n_mlp[l, :].rearrange("(k p) -> p k", p=128), writes=[b_const],
                      append=True, allow_slow_non_contiguous=True)
                for jt in range(3):
                    S.dma("sp", CW[:, l, :, jt], conv_w[l, jt, :].rearrange("(c p) -> p c", p=128),
                          writes=[b_const], append=True, allow_slow_non_contiguous=True)
            S.op("pool", OP("memset", ONES[:, :], 1.0), writes=[b_const])
            with ExitStack() as st0:
                stg = [sb(st0, f"stg{i}", [128, 2048], F32) for i in range(4)]
                stb = [sb(st0, f"stb{i}", [128, 2048], BF16) for i in range(4)]
                b_stg = [Buf(f"stg{i}") for i in range(4)]
                b_stb = [Buf(f"stb{i}") for i in range(4)]
                tiles_ = [(r0, c0, n_) for r0 in range(0, D, 128) for (c0, n_) in ((0, 2048), (2048, 2048), (4096, 1348))]

                def ld(i):
                    r0, c0, n_ = tiles_[i]
                    S.dma("sp", stg[i % 4][:, 0:n_], w_in[0, r0:r0 + 128, c0:c0 + n_], writes=[b_stg[i % 4]])
                for i in range(4):
                    ld(i)
                for i, (r0, c0, n_) in enumerate(tiles_):
                    k = i % 4
                    if i % 2 == 0:
                        S.op("dve", OP("tensor_copy", stb[k][:, 0:n_], stg[k][:, 0:n_]), reads=[b_stg[k]],
                             writes=[b_stb[k]])
                    else:
                        S.op("act", OP("activation", stb[k][:, 0:n_], stg[k][:, 0:n_], AF.Copy), reads=[b_stg[k]],
                             writes=[b_stb[k]])
                    S.dma("sp", WinB[0][r0:r0 + 128, c0:c0 + n_], stb[k][:, 0:n_], reads=[b_stb[k]],
                          writes=[b_W[("in", 0)]], append=True)
                    if i + 4 < len(tiles_):
                        ld(i + 4)
            S.flush(block)

        if stop_after == "init":
            return nc
        def conversion_thunks():
            th = []
            ncv = 0
            for l_ in range(nlayers):
                for (nm, src, dst, rows, rb) in (("in", w_in, WinB, D, 256), ("ao", w_ao, WaoB, 512, 512),
                                                 ("co", w_co, WcoB, 512, 512), ("mx", w_mx, WmxB, D, 512),
                                                 ("up", w_up, WupB, D, 256), ("dn", w_dn, WdnB, HID, 512)):
                    if l_ == 0 and nm == "in":
                        continue
                    for r0 in range(0, rows, rb):
                        def cv(l_=l_, nm=nm, src=src, dst=dst, r0=r0, rb=rb, ncv=ncv):
                            S.dma("pool",
                                  dst[l_][r0:r0 + rb, :].rearrange("r c -> (r c)").rearrange("(a b) -> a b", a=16),
                                  src[l_, r0:r0 + rb, :].rearrange("r c -> (r c)").rearrange("(a b) -> a b", a=16),
                                  writes=[b_W[(nm, l_)]], append=True, fixed=ncv)
                        th.append(cv)
                        ncv += 1
            return th

        conv_th = conversion_thunks()

        def norm_group(bufs, xt, b_xt, gvec_ap, dst, b_dst, part="all"):
            XS2, SSQ, b_x2, b_ss = bufs
            if part in ("all", "stats"):
                for tau in range(4):
                    S.op("act", OP("activation", XS2[tau % 2][:, :], xt[:, tau, :], AF.Square,
                                   accum_out=SSQ[:, tau:tau + 1]), reads=[b_xt], writes=[b_x2[tau % 2], b_ss])
                S.op("dve", OP("tensor_scalar", SSQ[:, 4:8], SSQ[:, 0:4], 1.0 / D, EPS, ALU.mult, ALU.add),
                     reads=[b_ss], writes=[b_ss])
                S.op("act", OP("activation", SSQ[:, 8:12], SSQ[:, 4:8], AF.Sqrt), reads=[b_ss], writes=[b_ss])
                S.op("dve", OP("reciprocal", SSQ[:, 12:16], SSQ[:, 8:12]), reads=[b_ss], writes=[b_ss])
            if part == "stats":
                return
            for tau in range(4):
                x2, bx = XS2[tau % 2], b_x2[tau % 2]
                c0 = tau * 128
                S.op("dve", OP("tensor_scalar", x2[:, :], xt[:, tau, :], SSQ[:, 12 + tau:13 + tau], None, ALU.mult),
                     reads=[b_xt, b_ss], writes=[bx])
                for half in range(2):
                    bk = 6 + half
                    for k4 in range(4):
                        kc = half * 4 + k4
                        S.op("pe", OP("transpose", PS[bk][:, k4 * 128:(k4 + 1) * 128],
                                      x2[:, kc * 128:(kc + 1) * 128], IDF[:, :]),
                             reads=[bx, b_const], writes=[b_PS[bk]], sig=(k4 == 3))
                    for k4 in range(4):
                        kc = half * 4 + k4
                        S.op("act", OP("activation", dst[:, kc, c0:c0 + 128], PS[bk][:, k4 * 128:(k4 + 1) * 128],
                                       AF.Copy, scale=gvec_ap[:, kc:kc + 1]),
                             reads=[b_PS[bk], b_const], writes=[b_dst])

        class WStream:
            def __init__(self, st, nbuf, loads, live):
                self.ahead = nbuf - live
                self.bufs = [sb(st, f"WB{i}", [128, 4096], BF16) for i in range(nbuf)]
                self.bb = [Buf(f"WB{i}") for i in range(nbuf)]
                self.loads = loads
                self.issued = 0
                self.nbuf = nbuf

            def _issue(self):
                i = self.issued
                src, k, c, wbuf = self.loads[i]
                t = self.bufs[i % self.nbuf]
                dst = t[:, 0:k * c].rearrange("p (k c) -> p k c", k=k)
                S.dma("sp", dst, src, reads=[wbuf], writes=[self.bb[i % self.nbuf]])
                self.issued += 1

            def get(self, i):
                while self.issued < min(len(self.loads), i + self.ahead + 1):
                    self._issue()
                src, k, c, wbuf = self.loads[i]
                t = self.bufs[i % self.nbuf]
                return t[:, 0:k * c].rearrange("p (k c) -> p k c", k=k), self.bb[i % self.nbuf]

        def wsrc(tensor_l, c0, ncols, k0=0, nk=8):
            return tensor_l.ap().rearrange("(k p) c -> p k c", p=128)[:, k0:k0 + nk, c0:c0 + ncols]

        for l in range(nlayers):
            x_src, b_xsrc = (x_in, b_const) if l == 0 else (XRd, b_XRd)
            last = (l == nlayers - 1)
            ab = ExitStack()
            QT = sb(ab, "QT", [128, 4, TL], BF16)
            IQT = sb(ab, "IQT", [128, 2, TL], BF16)
            with ExitStack() as st, nc.Block() as block:
                CT = sb(st, "CT", [128, TL], F32)
                STb = sb(st, "STb", [128, TL], F32)
                XT = [sb(st, f"XTa{i}", [128, 4, D], F32) for i in range(2)]
                UTg = [sb(st, f"UTg{i}", [128, 8, 512], BF16) for i in range(2)]
                WSW = sb(st, "WSW", [128, 11, 8, 128], BF16)
                KS = sb(st, "KS", [128, 4, 512], BF16)
                IKS = sb(st, "IKS", [64, 512], BF16)
                VS = sb(st, "VS", [128, 4, 512], BF16)
                CS = [sb(st, f"CS{i}", [128, 512], F32) for i in range(2)]
                CHS = sb(st, "CHS", [128, 4, 4, 128], BF16)
                HS = sb(st, "HS", [128, 4, NT, 2], BF16)
                T1 = [sb(st, f"T1{i}", [128, 512], F32) for i in range(2)]
                T2 = [sb(st, f"T2{i}", [128, 512], F32) for i in range(2)]
                XS2 = [sb(st, f"XS2a{i}", [128, D], F32) for i in range(2)]
                SSQ = sb(st, "SSQ", [128, 16], F32)
                b_rope, b_XT, b_UTg, b_WSW = Buf("rope"), [Buf("XT0"), Buf("XT1")], [Buf("UTg0"), Buf("UTg1")], \
                    Buf("WSW")
                b_KS, b_IKS, b_VS, b_CS, b_CHS, b_HS = Buf("KS"), Buf("IKS"), Buf("VS"), [Buf("CS0"), Buf("CS1")], \
                    Buf("CHS"), Buf("HS")
                b_T1, b_T2 = [Buf("T10"), Buf("T11")], [Buf("T20"), Buf("T21")]
                b_x2a, b_ssa = [Buf("x2a0"), Buf("x2a1")], Buf("ssa")
                S.op("dve", OP("memset", CT[:, :], 1.0), writes=[b_rope])
                S.op("dve", OP("memset", STb[:, :], 0.0), writes=[b_rope])
                for r0 in (0, 64):
                    S.dma("sp", CT[r0:r0 + 16, :], ropeC[:, :], writes=[b_rope], append=True)
                    S.dma("sp", STb[r0:r0 + 16, :], ropeS[:, :], writes=[b_rope], append=True)
                loads = [(wsrc(WinB[l], C_Q, 512), 8, 512, b_W[("in", l)]),
                         (wsrc(WinB[l], C_K, 512), 8, 512, b_W[("in", l)]),
                         (wsrc(WinB[l], C_IQ, 324), 8, 324, b_W[("in", l)])]
                for G in range(4):
                    loads += [(wsrc(WinB[l], C_Q, 512), 8, 512, b_W[("in", l)]),
                              (wsrc(WinB[l], C_K, 512), 8, 512, b_W[("in", l)]),
                              (wsrc(WinB[l], C_V, 512), 8, 512, b_W[("in", l)]),
                              (wsrc(WinB[l], C_IQ, 324), 8, 324, b_W[("in", l)]),
                              (wsrc(WinB[l], C_CC, 512), 8, 512, b_W[("in", l)]),
                              (wsrc(WinB[l], C_CH, 512), 8, 512, b_W[("in", l)])]
                ws = WStream(st, 4, loads, 2)
                xsp = XSp[l]
                swi = [0]
                bank = [0]

                def fm_proj(Wt, b_w, c0, nf, utg, b_utg, bk):
                    for kc in range(8):
                        S.op("pe", OP("matmul", PS[bk][0:nf, :], Wt[:, kc, c0:c0 + nf], utg[:, kc, :],
                                                             start=(kc == 0), stop=(kc == 7)),
                             reads=[b_w, b_utg], writes=[b_PS[bk]], sig=(kc == 7))

                ci = 0
                for li, nch_, in ((0, 4), (1, 4), (2, 3)):
                    Wt0, b_w0 = ws.get(li)
                    for c in range(nch_):
                        nf = 64 if (li == 2 and c == 2) else 128
                        c0 = c * 128
                        S.op("dve", OP("tensor_copy", WSW[:, ci, :, 0:nf], Wt0[:, :, c0:c0 + nf]), reads=[b_w0],
                             writes=[b_WSW])
                        for hb in range(0, nf, 64):
                            S.op("dve", OP("tensor_copy", WSW[:, ci, :, hb:hb + 8], Wt0[:, :, c0 + hb + 8:c0 + hb + 16]),
                                 reads=[b_w0], writes=[b_WSW])
                            S.op("dve", OP("tensor_copy", WSW[:, ci, :, hb + 8:hb + 16], Wt0[:, :, c0 + hb:c0 + hb + 8]),
                                 reads=[b_w0], writes=[b_WSW])
                        ci += 1

                def rope_chunk(Wt, b_w, c0, nf, utg, b_utg, dst_ap, b_dst, g0, ci):
                    si = swi[0] % 2
                    swi[0] += 1
                    bq = bank[0] % 6
                    bs = (bank[0] + 1) % 6
                    bank[0] += 2
                    fm_proj(Wt, b_w, c0, nf, utg, b_utg, bq)
                    fm_proj(WSW[:, ci, :, :], b_WSW, 0, nf, utg, b_utg, bs)
                    S.op("dve", OP("tensor_tensor", T1[si][0:nf, :], PS[bs][0:nf, :], STb[0:nf, g0:g0 + 512], ALU.mult),
                         reads=[b_PS[bs], b_rope], writes=[b_T1[si]])
                    S.op("dve", OP("tensor_tensor", T2[si][0:nf, :], PS[bq][0:nf, :], CT[0:nf, g0:g0 + 512], ALU.mult),
                         reads=[b_PS[bq], b_rope], writes=[b_T2[si]])
                    S.op("dve", OP("tensor_tensor", dst_ap, T1[si][0:nf, :], T2[si][0:nf, :], ALU.add),
                         reads=[b_T1[si], b_T2[si]], writes=[b_dst])

                def xload(G_):
                    S.dma("sp", XT[G_ % 2][:, :, :],
                          x_src[G_ * 512:G_ * 512 + 512, :].rearrange("(t p) d -> p t d", p=128),
                          reads=[b_xsrc], writes=[b_XT[G_ % 2]])

                def head_norm(G_):
                    k_ = G_ % 2
                    norm_group((XS2, SSQ, b_x2a, b_ssa), XT[k_], b_XT[k_], GM[:, l, :], UTg[k_], b_UTg[k_],
                               part=("all" if G_ == 0 else "apply"))
                    S.dma("sp", UTd.ap().rearrange("(k p) t -> p k t", p=128)[:, :, G_ * 512:G_ * 512 + 512],
                          UTg[k_][:, :, :], reads=[b_UTg[k_]], writes=[b_UTd], append=True)

                xload(0)
                head_norm(0)
                for G in range(4):
                    g0 = G * 512
                    xb = G % 2
                    xt, utg = XT[xb], UTg[xb]
                    if G < 3:
                        xload(G + 1)
                    S.mute = CUT < 2
                    Wt, b_w = ws.get(3 + G * 6 + 0)
                    for c in range(4):
                        rope_chunk(Wt, b_w, c * 128, 128, utg, b_UTg[xb], QT[:, c, g0:g0 + 512], b_QT, g0, c)
                    S.mute = CUT < 3
                    Wt, b_w = ws.get(3 + G * 6 + 1)
                    for c in range(4):
                        rope_chunk(Wt, b_w, c * 128, 128, utg, b_UTg[xb], KS[:, c, :], b_KS, g0, 4 + c)
                    for hh in range(2):
                        S.dma("sp", xsp[hh][0:256, :].rearrange("(c p) t -> p c t", p=128)[:, :, g0:g0 + 512],
                              KS[:, 2 * hh:2 * hh + 2, :], reads=[b_KS], writes=[b_XSd[l]], append=True)
                    if G < 3:
                        norm_group((XS2, SSQ, b_x2a, b_ssa), XT[(G + 1) % 2], b_XT[(G + 1) % 2], GM[:, l, :],
                                   UTg[(G + 1) % 2], b_UTg[(G + 1) % 2], part="stats")
                    S.mute = CUT < 4
                    Wt, b_w = ws.get(3 + G * 6 + 2)
                    for tau in range(4):
                        bk = bank[0] % 6
                        bank[0] += 1
                        for kc in range(8):
                            S.op("pe", OP("matmul",
                                PS[bk][:, :], utg[:, kc, tau * 128:(tau + 1) * 128], Wt[:, kc, :],
                                start=(kc == 0), stop=(kc == 7)),
                                 reads=[b_w, b_UTg[xb]], writes=[b_PS[bk]], sig=(kc == 7))
                        S.op("act", OP("activation", VS[:, tau, :], PS[bk][:, :], AF.Copy),
                             reads=[b_PS[bk]], writes=[b_VS])
                    vview = xsp[2 + G // 2][0:256, :].rearrange("r (q c) -> (r q) c", q=4)
                    S.dma("sp", vview[(G % 2) * 512:(G % 2) * 512 + 512, :].rearrange("(t p) c -> p t c", p=128),
                          VS[:, :, :],
                          reads=[b_VS], writes=[b_XSd[l]], append=True)
                    S.mute = CUT < 5
                    Wt, b_w = ws.get(3 + G * 6 + 3)
                    for c in range(2):
                        rope_chunk(Wt, b_w, c * 128, 128, utg, b_UTg[xb], IQT[:, c, g0:g0 + 512], b_IQT, g0, 8 + c)
                    rope_chunk(Wt, b_w, 256, 64, utg, b_UTg[xb], IKS[:, :], b_IKS, g0, 10)
                    S.dma("sp", xsp[1][256:320, g0:g0 + 512], IKS[:, :], reads=[b_IKS], writes=[b_XSd[l]], append=True)
                    for tau in range(4):
                        j = G * 4 + tau
                        bk = bank[0] % 6
                        bank[0] += 1
                        for kc in range(8):
                            S.op("pe", OP("matmul",
                                PS[bk][:, 0:4], utg[:, kc, tau * 128:(tau + 1) * 128], Wt[:, kc, 320:324],
                                start=(kc == 0), stop=(kc == 7)),
                                 reads=[b_w, b_UTg[xb]], writes=[b_PS[bk]], sig=(kc == 7))
                        S.op("act", OP("activation", AW[:, j, :], PS[bk][:, 0:4], AF.Abs, scale=1.0 / 16.0),
                             reads=[b_PS[bk]], writes=[b_AW])
                        S.op("act", OP("activation", SG[:, j, :], PS[bk][:, 0:4], AF.Sign),
                             reads=[b_PS[bk]], writes=[b_SG])
                    if G < 3:
                        head_norm(G + 1)
                    S.mute = CUT < 6
                    WtC, b_wC = ws.get(3 + G * 6 + 4)
                    WtH, b_wH = ws.get(3 + G * 6 + 5)
                    for c in range(4):
                        bc = bank[0] % 6
                        bh = (bank[0] + 1) % 6
                        bank[0] += 2
                        fm_proj(WtC, b_wC, c * 128, 128, utg, b_UTg[xb], bc)
                        fm_proj(WtH, b_wH, c * 128, 128, utg, b_UTg[xb], bh)
                        S.op("act", OP("activation", CS[c % 2][:, :], PS[bc][:, :], AF.Copy),
                             reads=[b_PS[bc]], writes=[b_CS[c % 2]])
                        S.op("dve", OP("tensor_tensor",
                            CHS[:, c, :, :], CS[c % 2][:, :].rearrange("p (t w) -> p t w", t=4),
                            PS[bh][:, :].rearrange("p (t w) -> p t w", t=4), ALU.mult),
                             reads=[b_PS[bh], b_CS[c % 2]], writes=[b_CHS])
                    chv = CHd.ap().rearrange("(c p) (j w) -> p c j w", p=128, w=130)
                    for c in range(4):
                        S.dma("sp", chv[:, c, G * 4:G * 4 + 4, 2:130], CHS[:, c, :, :], reads=[b_CHS], writes=[b_CHd],
                              append=True)
                    S.op("dve", OP("tensor_copy", HS[:, :, G * 4:G * 4 + 4, :], CHS[:, :, :, 126:128]),
                         reads=[b_CHS], writes=[b_HS])
                S.mute = CUT < 7
                hv = xsp[3][256:264, :].rearrange("r (a b) -> (r a) b", b=32).rearrange("(c p) b -> p c b", p=128)
                S.dma("sp", hv, HS[:, :, :, :].rearrange("p c j w -> p c (j w)"), reads=[b_HS], writes=[b_XSd[l]],
                      append=True)
                S.mute = False
                if debug and l == 0:
                    S.dma("sp", dbg["d_ut"][:, :], UTd[:, :], reads=[b_UTd], writes=[b_dbg], append=True)
                    S.dma("sp", dbg["d_qt"].ap().rearrange("(c p) t -> p c t", p=128), QT[:, :, :], reads=[b_QT],
                          writes=[b_dbg], append=True)
                    S.dma("sp", dbg["d_iqt"].ap().rearrange("(c p) t -> p c t", p=128), IQT[:, :, :], reads=[b_IQT],
                          writes=[b_dbg], append=True)
                    S.dma("sp", dbg["d_aw"][:, :], AW[:, :, :].rearrange("p j h -> p (j h)"), reads=[b_AW],
                          writes=[b_dbg], append=True)
                    S.dma("sp", dbg["d_sg"][:, :], SG[:, :, :].rearrange("p j h -> p (j h)"), reads=[b_SG],
                          writes=[b_dbg], append=True)
                S.mute = CUT < 8
                for kk in range(4):
                    S.collective(xsp[kk].ap().opt(), XGp[l][kk].ap().opt(), reads=[b_XSd[l]], writes=[b_XGd[l]])
                S.mute = False
                S.barrier()
                S.flush(block)

            if stop_after == f"A{l}":
                ab.close()
                return nc
            with ExitStack() as st, nc.Block() as block:
                KT = sb(st, "KT", [128, 4, T], BF16)
                VA = sb(st, "VA", [128, 32, 512], BF16)
                IKT = sb(st, "IKT", [128, T], BF16)
                SC1 = sb(st, "SC", [128, T], F32)
                SCs = [SC1, SC1]
                MB = sb(st, "MB", [128, 2, T], BF16)
                MTs = [sb(st, f"MT{i}", [128, 32, 256], BF16) for i in range(2)]
                RR = [[sb(st, f"RR{i}{h}", [128, 512], BF16) for h in range(4)] for i in range(2)]
                PT = [sb(st, f"PT{i}", [128, 512], BF16) for i in range(4)]
                DGs = [sb(st, f"DG{i}", [128, 4, 128], BF16) for i in range(2)]
                CBd = sb(st, "CBd", [128, 8], F32)
                CBa = sb(st, "CBa", [128, 4], F32)
                CBf = sb(st, "CBf", [128, 2], F32)
                RC = [sb(st, f"RC{i}", [128, 512], F32) for i in range(2)]
                QBD = [sb(st, f"QBD{i}", [128, 4, 512], BF16) for i in range(2)]
                b_QBD = [Buf("QBD0"), Buf("QBD1")]
                for i_ in range(2):
                    S.op("pool", OP("memset", QBD[i_][:, :, :], 0.0), writes=[b_QBD[i_]])
                b_KT, b_VA, b_IKT, b_MTs = Buf("KT"), Buf("VA"), Buf("IKT"), [Buf("MT0"), Buf("MT1")]
                b_sc1 = Buf("SC")
                b_SC, b_MB, b_DG = [b_sc1, b_sc1], [Buf("MB0"), Buf("MB1")], [Buf("DG0"), Buf("DG1")]
                b_JKd, b_JKa = b_MB[0], b_MB[1]
                b_CBd2 = [Buf("CBd0"), Buf("CBd1")]
                b_RR = [[Buf(f"RR{i}{h}") for h in range(4)] for i in range(2)]
                b_PT, b_CBd, b_CBa, b_CBf, b_RC = [Buf("PT0"), Buf("PT1"), Buf("PT2"), Buf("PT3")], Buf("CBd"), Buf("CBa"), \
                    [Buf("CBf0"), Buf("CBf1")], [Buf("RC0"), Buf("RC1")]
                xgp = XGp[l]
                for pp in range(2):
                    for hp in range(4):
                        kk = hp // 2
                        r0 = pp * PR[kk] + (hp % 2) * 128
                        S.dma("sp", KT[:, hp, :].rearrange("p (j q i) -> p j q i", q=2, i=128)[:, :, pp, :],
                              xgp[kk][r0:r0 + 128, :].rearrange("p (j i) -> p j i", i=128),
                              reads=[b_XGd[l]], writes=[b_KT], append=True)
                    for r0 in (0, 64):
                        S.dma("sp", IKT[r0:r0 + 64, :].rearrange("p (j q i) -> p j q i", q=2, i=128)[:, :, pp, :],
                              xgp[1][pp * 320 + 256:pp * 320 + 320, :].rearrange("p (j i) -> p j i", i=128),
                              reads=[b_XGd[l]], writes=[b_IKT], append=True)
                for j in range(NT):
                    for pp in range(2):
                        kk = 2 + j // 8
                        vview = xgp[kk][pp * PR[kk]:pp * PR[kk] + 256, :].rearrange("r (q c) -> (r q) c", q=4)
                        jj = j % 8
                        S.dma("sp", VA[:, 2 * j + pp, :], vview[jj * 128:(jj + 1) * 128, :], reads=[b_XGd[l]],
                              writes=[b_VA], append=True)
                lrot = [0]

                def ib_units(g):
                    nch = g + 1
                    N = 512 * nch
                    nkb = 4 * nch
                    MT, b_MT = MTs[g % 2], b_MTs[g % 2]
                    U = []
                    wK = BIS_R / (2.0 ** BIS_ITERS)
                    for tau in range(2):
                        j = 2 * g + tau
                        SC, b_sc, DG, b_dg = SC1, b_sc1, DGs[tau], b_DG[tau]

                        def dg_unit(j=j, DG=DG, b_dg=b_dg):
                            for h in range(4):
                                S.op("pool", OP("tensor_scalar", DG[:, h, :], IDB[:, :], SG[:, j, h:h + 1], 1.0,
                                               ALU.mult, ALU.mult), reads=[b_SG, b_const], writes=[b_dg])
                        U.append(dg_unit)
                        for c in range(nch):
                            ri = c % 2
                            for h in range(4):
                                def lg_unit(c=c, j=j, h=h, ri=ri):
                                    pr = (h % 2) * 64
                                    bk = 6 + lrot[0] % 2
                                    lrot[0] += 1
                                    S.op("pe", OP("matmul", PS[bk][:, :], IQT[pr:pr + 64, h // 2, j * 128:(j + 1) * 128],
                                                  IKT[pr:pr + 64, c * 512:(c + 1) * 512], start=True, stop=True),
                                         reads=[b_IQT, b_IKT], writes=[b_PS[bk]])
                                    S.op("act", OP("activation", RR[ri][h][:, :], PS[bk][:, :], AF.Relu,
                                                   scale=AW[:, j, h:h + 1]),
                                         reads=[b_PS[bk], b_AW], writes=[b_RR[ri][h]])
                                U.append(lg_unit)

                            def cmb_unit(c=c, tau=tau, DG=DG, b_dg=b_dg, ri=ri):
                                bk = 6 + lrot[0] % 2
                                lrot[0] += 1
                                for h in range(4):
                                    S.op("pe", OP("matmul", PS[bk][:, :], DG[:, h, :], RR[ri][h][:, :],
                                                  start=(h == 0), stop=(h == 3)),
                                         reads=[b_dg, b_RR[ri][h]], writes=[b_PS[bk]], sig=(h == 3))
                                if c == nch - 1:
                                    S.op("dve", OP("tensor_tensor", SC1[:, c * 512:(c + 1) * 512], PS[bk][:, :],
                                                   ADM[:, tau, :], ALU.add),
                                         reads=[b_PS[bk], b_const], writes=[b_sc1])
                                else:
                                    S.op("act", OP("activation", SC1[:, c * 512:(c + 1) * 512], PS[bk][:, :], AF.Copy),
                                         reads=[b_PS[bk]], writes=[b_sc1])
                            U.append(cmb_unit)
                        if j == 0:
                            U.append(lambda: S.op("dve", OP("memset", CBf[:, 0:1], -10000.0), writes=[b_CBf[0]]))
                        else:
                            U.append(lambda: S.op("dve", OP("memset", CBd[:, 0:1], 0.0), writes=[b_CBd]))
                            for k in range(1, BIS_ITERS + 1):
                                def bis_d(k=k):
                                    wk = BIS_R / (2.0 ** k)
                                    S.op("dve", OP("tensor_scalar", MB[:, 1, 0:N], SC1[:, 0:N], CBd[:, 0:1], None,
                                                   ALU.is_ge, ALU.add, accum_out=CBd[:, 1:2]),
                                         reads=[b_sc1, b_CBd], writes=[b_MB[1], b_CBd])
                                    S.op("dve", OP("tensor_scalar", CBd[:, 2:3], CBd[:, 1:2], 255.5, -0.5,
                                                   ALU.is_ge, ALU.add), reads=[b_CBd], writes=[b_CBd])
                                    S.op("dve", OP("scalar_tensor_tensor", CBd[:, 0:1], CBd[:, 2:3], 2.0 * wk,
                                                   CBd[:, 0:1], ALU.mult, ALU.add), reads=[b_CBd], writes=[b_CBd])
                                U.append(bis_d)
                            U.append(lambda tau=tau: S.op(
                                "dve", OP("tensor_scalar", CBf[:, tau:tau + 1], CBd[:, 0:1], -wK, None, ALU.add),
                                reads=[b_CBd], writes=[b_CBf[tau]]))
                        U.append(lambda tau=tau: S.op(
                            "dve", OP("tensor_scalar", MB[:, tau, 0:N], SC1[:, 0:N], CBf[:, tau:tau + 1], None,
                                      ALU.is_ge), reads=[b_sc1, b_CBf[tau]], writes=[b_MB[tau]]))
                    for k2 in range(nkb // 2):
                        def tr_unit(k2=k2):
                            bk = 6 + lrot[0] % 2
                            lrot[0] += 1
                            for ee in range(2):
                                kb = 2 * k2 + ee
                                for tau in range(2):
                                    S.op("pe", OP("matmul", PS[bk][:, ee * 256 + tau * 128:ee * 256 + (tau + 1) * 128],
                                                  MB[:, tau, kb * 128:(kb + 1) * 128], IDB[:, :], start=True,
                                                  stop=True),
                                         reads=[b_MB[tau], b_const], writes=[b_PS[bk]], sig=(ee == 1 and tau == 1))
                            S.op("act", OP("activation", MT[:, 2 * k2:2 * k2 + 2, :].rearrange("p a t -> p (a t)"),
                                           PS[bk][:, :], AF.Copy), reads=[b_PS[bk]], writes=[b_MT])
                        U.append(tr_unit)
                    return U

                def qbd_build(g):
                    t0 = 2 * g * 128
                    qb, bq = QBD[g % 2], b_QBD[g % 2]
                    for hp in range(4):
                        S.op("act", OP("activation", qb[0:64, hp, 0:256], QT[0:64, hp, t0:t0 + 256], AF.Copy),
                             reads=[b_QT], writes=[bq])
                        S.op("act", OP("activation", qb[64:128, hp, 256:512], QT[64:128, hp, t0:t0 + 256], AF.Copy),
                             reads=[b_QT], writes=[bq])

                def att_units(g):
                    nkb = 4 * (g + 1)
                    MT, b_MT = MTs[g % 2], b_MTs[g % 2]
                    qb, bq = QBD[g % 2], b_QBD[g % 2]
                    units = [(hp, kb) for hp in range(4) for kb in range(nkb)]

                    def stage12(i):
                        hp, kb = units[i]
                        bs, pi = i % 2, i % 4
                        S.op("pe", OP("matmul", PS[bs][:, :], KT[:, hp, kb * 128:(kb + 1) * 128], qb[:, hp, :],
                                      start=True, stop=True),
                             reads=[b_KT, bq], writes=[b_PS[bs]])
                        S.op("act", OP("activation", PT[pi][:, :], PS[bs][:, :], AF.Exp, scale=0.125),
                             reads=[b_PS[bs]], writes=[b_PT[pi]])
                        S.op("pool", OP("tensor_tensor", PT[pi][:, :].rearrange("p (a t) -> p a t", a=2),
                                        PT[pi][:, :].rearrange("p (a t) -> p a t", a=2),
                                        MT[:, kb:kb + 1, :].to_broadcast([128, 2, 256]), ALU.mult),
                             reads=[b_PT[pi], b_MT], writes=[b_PT[pi]])

                    def stage3(i):
                        hp, kb = units[i]
                        pi = i % 4
                        bo, br = 2 + hp % 2, 4 + hp % 2
                        S.op("pe", OP("matmul", PS[bo][:, :], VA[:, kb, hp * 128:(hp + 1) * 128], PT[pi][:, :],
                                      start=(kb == 0), stop=(kb == nkb - 1)),
                             reads=[b_VA, b_PT[pi]], writes=[b_PS[bo]], sig=(kb == nkb - 1))
                        S.op("pe", OP("matmul", PS[br][:, :], ONES[:, :], PT[pi][:, :],
                                      start=(kb == 0), stop=(kb == nkb - 1)),
                             reads=[b_const, b_PT[pi]], writes=[b_PS[br]])
                    return units, stage12, stage3

                def att_norm(g, hp):
                    t0 = 2 * g * 128
                    bo, br = 2 + hp % 2, 4 + hp % 2
                    ri = hp % 2
                    for e2 in range(2):
                        pr, c0 = e2 * 64, e2 * 256
                        S.op("dve", OP("reciprocal", RC[ri][pr:pr + 64, c0:c0 + 256], PS[br][pr:pr + 64, c0:c0 + 256]),
                             reads=[b_PS[br]], writes=[b_RC[ri]])
                        S.op("dve", OP("tensor_tensor", AT[pr:pr + 64, hp, t0:t0 + 256], PS[bo][pr:pr + 64, c0:c0 + 256],
                                       RC[ri][pr:pr + 64, c0:c0 + 256], ALU.mult),
                             reads=[b_PS[bo], b_RC[ri]], writes=[b_AT])

                for u in ib_units(0):
                    u()
                qbd_build(0)
                for g in range(8):
                    ibu = ib_units(g + 1) if g < 7 else []
                    n = len(ibu)
                    units, stage12, stage3 = att_units(g)
                    U = len(units)
                    per_h = U // 4
                    ib_done = 0
                    SK = 2
                    for i in range(U + SK):
                        if i < U:
                            stage12(i)
                        if i >= SK:
                            i3 = i - SK
                            stage3(i3)
                            tgt = min(n, ((i3 + 1) * n * 5 + 3 * U - 1) // (3 * U))
                            for u in ibu[ib_done:tgt]:
                                u()
                            ib_done = tgt
                            hprev, kbprev = units[i3]
                            if kbprev == per_h - 1:
                                att_norm(g, hprev)
                            if conv_th and (i3 % 6 == 0):
                                conv_th.pop(0)()
                        if i == U // 2 and g < 7:
                            qbd_build(g + 1)
                    for u in ibu[ib_done:]:
                        u()
                while conv_th:
                    conv_th.pop(0)()
                if debug and l == 0:
                    S.dma("sp", dbg["d_at"].ap().rearrange("(c p) t -> p c t", p=128), AT[:, :, :], reads=[b_AT],
                          writes=[b_dbg], append=True)
                S.barrier()
                S.flush(block)

            ab.close()
            if stop_after == f"B{l}":
                return nc
            with ExitStack() as st, nc.Block() as block:
                XT = sb(st, "XTc", [128, 4, D], F32)
                GF = sb(st, "GF", [128, D], F32)
                b_GF = Buf("GF")
                if last:
                    S.dma("sp", GF[:, :], n_fin.ap().partition_broadcast(128), writes=[b_GF])
                UTgL = [sb(st, f"UTc{i}", [128, 8, 512], BF16) for i in range(2)]
                U2 = sb(st, "U2", [128, 8, 512], BF16)
                CHBL = [sb(st, f"CHB{i}", [128, 4, 4, 130], BF16) for i in range(2)]
                HAL = [sb(st, f"HA{i}", [128, 4, 4, 2], BF16) for i in range(2)]
                HBL = [sb(st, f"HB{i}", [128, 4, 4, 2], BF16) for i in range(2)]
                b_UTgL, b_CHBL = [Buf("UTc0"), Buf("UTc1")], [Buf("CHB0"), Buf("CHB1")]
                b_HAL, b_HBL = [Buf("HA0"), Buf("HA1")], [Buf("HB0"), Buf("HB1")]
                HT1 = sb(st, "HT1", [128, 4, 4, 2], F32)
                CV = sb(st, "CV", [128, 4, 128], F32)
                ZT = sb(st, "ZT", [128, 4, 512], BF16)
                SG0 = sb(st, "SG0", [128, 512], F32)
                SG1 = sb(st, "SG1", [128, 512], F32)
                M1 = sb(st, "M1", [128, 512], F32)
                M2 = sb(st, "M2", [128, 512], F32)
                MG = sb(st, "MG", [128, 8, 512], BF16)
                HT = sb(st, "HT", [128, 32, 512], BF16)
                RL = [sb(st, f"RL{i}", [128, 512], F32) for i in range(2)]
                XS2 = [sb(st, f"XS2c{i}", [128, D], F32) for i in range(2)]
                SSQ = sb(st, "SSQc", [128, 16], F32)
                b_x2c, b_ssc = [Buf("x2c0"), Buf("x2c1")], Buf("ssc")
                b_XT, b_UTg, b_U2, b_CHB, b_HA, b_HB, b_HT1, b_CV, b_ZT = (Buf(n) for n in (
                    "XTc", "UTc", "U2", "CHB", "HA", "HB", "HT1", "CV", "ZT"))
                b_SG0, b_SG1, b_M1, b_M2, b_MG, b_HT, b_nt, b_OT = (Buf(n) for n in (
                    "SG0", "SG1", "M1", "M2", "MG", "HT", "ntc", "OT"))
                b_RL = [Buf("RL0"), Buf("RL1")]
                loads = []
                for G in range(4):
                    loads += [(wsrc(WinB[l], C_CB, 512), 8, 512, b_W[("in", l)])]
                    for fh in range(2):
                        loads += [(wsrc(WaoB[l], fh * 512, 512, 0, 4), 4, 512, b_W[("ao", l)]),
                                  (wsrc(WcoB[l], fh * 512, 512, 0, 4), 4, 512, b_W[("co", l)]),
                                  (wsrc(WinB[l], C_G + fh * 512, 512), 8, 512, b_W[("in", l)]),
                                  (wsrc(WinB[l], C_G + 1024 + fh * 512, 512), 8, 512, b_W[("in", l)])]
                    loads += [(wsrc(WmxB[l], hf * 512, 512), 8, 512, b_W[("mx", l)]) for hf in range(2)]
                    loads += [(wsrc(WupB[l], i * 512, 512), 8, 512, b_W[("up", l)]) for i in range(8)]
                    loads += [(wsrc(WdnB[l], hf * 512, 512, hg * 8, 8), 8, 512, b_W[("dn", l)])
                              for hf in range(2) for hg in range(4)]
                NLG = len(loads) // 4
                ws = WStream(st, 7, loads, 4)
                xgp = XGp[l]
                chv = CHd.ap().rearrange("(c p) (j w) -> p c j w", p=128, w=130)

                def halo_view(pp):
                    base = pp * 264 + 256
                    return xgp[3][base:base + 8, :].rearrange("r (a b) -> (r a) b", b=32).rearrange(
                        "(c p) (j w) -> p c j w", p=128, w=2)

                def prefetch(G):
                    g0_ = G * 512
                    k_ = G % 2
                    S.dma("sp", UTgL[k_][:, :, :], UTd.ap().rearrange("(k p) t -> p k t", p=128)[:, :, g0_:g0_ + 512],
                          reads=[b_UTd], writes=[b_UTgL[k_]])
                    for c in range(4):
                        S.dma("sp", CHBL[k_][:, c, :, 2:130], chv[:, c, G * 4:G * 4 + 4, 2:130], reads=[b_CHd],
                              writes=[b_CHBL[k_]], append=(c > 0))
                    for c in range(4):
                        S.dma("sp", HAL[k_][:, c, :, :], halo_view(0)[:, c, G * 4:G * 4 + 4, :], reads=[b_XGd[l]],
                              writes=[b_HAL[k_]], append=(c > 0))
                    S.op("pool", OP("memset", HBL[k_][:, :, :, :], 0.0), writes=[b_HBL[k_]])
                    for c in range(4):
                        if G == 0:
                            S.dma("sp", HBL[k_][:, c, 1:4, :], halo_view(1)[:, c, 0:3, :], reads=[b_XGd[l]],
                                  writes=[b_HBL[k_]], append=True)
                        else:
                            S.dma("sp", HBL[k_][:, c, :, :], halo_view(1)[:, c, G * 4 - 1:G * 4 + 3, :],
                                  reads=[b_XGd[l]], writes=[b_HBL[k_]], append=True)

                prefetch(0)
                for G in range(4):
                    g0 = G * 512
                    wi = G * NLG
                    UTg, b_UTg, CHB, b_CHB = UTgL[G % 2], b_UTgL[G % 2], CHBL[G % 2], b_CHBL[G % 2]
                    HA, b_HA, HB, b_HB = HAL[G % 2], b_HAL[G % 2], HBL[G % 2], b_HBL[G % 2]
                    S.dma("sp", XT[:, :, :], x_src[g0:g0 + 512, :].rearrange("(t p) d -> p t d", p=128),
                          reads=[b_xsrc], writes=[b_XT])
                    S.op("dve", OP("tensor_scalar", HT1[:, :, :, :], HA[:, :, :, :], SEL[:, 0:1], None, ALU.mult),
                         reads=[b_HA, b_const], writes=[b_HT1])
                    S.op("dve", OP("scalar_tensor_tensor",
                        CHB[:, :, :, 0:2].rearrange("p c j w -> p (c j) w"),
                        HB[:, :, :, :].rearrange("p c j w -> p (c j) w"), SEL[:, 1:2],
                        HT1[:, :, :, :].rearrange("p c j w -> p (c j) w"), ALU.mult, ALU.add),
                         reads=[b_HB, b_HT1, b_const, b_CHB], writes=[b_CHB])
                    Wt, b_w = ws.get(wi + 0)
                    for c in range(4):
                        bk = c % 2
                        for kc in range(8):
                            S.op("pe", OP("matmul",
                                PS[bk][:, :], Wt[:, kc, c * 128:(c + 1) * 128], UTg[:, kc, :], start=(kc == 0),
                                stop=(kc == 7)), reads=[b_w, b_UTg], writes=[b_PS[bk]], sig=(kc == 7))
                        S.op("dve", OP("tensor_scalar", CV[:, :, :], CHB[:, c, :, 2:130], CW[:, l, c, 2:3],
                                                                   None, ALU.mult),
                             reads=[b_CHB, b_const], writes=[b_CV])
                        for jt in (1, 0):
                            S.op("dve", OP("scalar_tensor_tensor",
                                CV[:, :, :], CHB[:, c, :, jt:jt + 128], CW[:, l, c, jt:jt + 1], CV[:, :, :], ALU.mult,
                                ALU.add), reads=[b_CHB, b_const, b_CV], writes=[b_CV])
                        S.op("dve", OP("tensor_tensor",
                            ZT[:, c, :], PS[bk][:, :], CV[:, :, :].rearrange("p t w -> p (t w)"), ALU.mult),
                             reads=[b_PS[bk], b_CV], writes=[b_ZT])
                    for f in range(8):
                        if f % 4 == 0:
                            fh = f // 4
                            Wa, b_wa = ws.get(wi + 1 + 4 * fh)
                            Wc, b_wc = ws.get(wi + 2 + 4 * fh)
                            Wg0, b_wg0 = ws.get(wi + 3 + 4 * fh)
                            Wg1, b_wg1 = ws.get(wi + 4 + 4 * fh)
                        fo = (f % 4) * 128
                        ba, bc_, bg0, bg1 = (0, 1, 2, 3) if f % 2 == 0 else (4, 5, 6, 7)
                        for kc in range(4):
                            S.op("pe", OP("matmul",
                                PS[ba][:, :], Wa[:, kc, fo:fo + 128], AT[:, kc, g0:g0 + 512],
                                start=(kc == 0), stop=(kc == 3)), reads=[b_wa, b_AT], writes=[b_PS[ba]],
                                 sig=(kc == 3))
                        for kc in range(4):
                            S.op("pe", OP("matmul",
                                PS[bc_][:, :], Wc[:, kc, fo:fo + 128], ZT[:, kc, :],
                                start=(kc == 0), stop=(kc == 3)), reads=[b_wc, b_ZT], writes=[b_PS[bc_]],
                                 sig=(kc == 3))
                        for (Wg, b_wg, bg) in ((Wg0, b_wg0, bg0), (Wg1, b_wg1, bg1)):
                            for kc in range(8):
                                S.op("pe", OP("matmul",
                                    PS[bg][:, :], Wg[:, kc, fo:fo + 128], UTg[:, kc, :], start=(kc == 0),
                                    stop=(kc == 7)), reads=[b_wg, b_UTg], writes=[b_PS[bg]], sig=(kc == 7))
                        S.op("act", OP("activation", SG0[:, :], PS[bg0][:, :], AF.Sigmoid),
                             reads=[b_PS[bg0]], writes=[b_SG0])
                        S.op("act", OP("activation", SG1[:, :], PS[bg1][:, :], AF.Sigmoid),
                             reads=[b_PS[bg1]], writes=[b_SG1])
                        S.op("dve", OP("tensor_tensor", M1[:, :], PS[ba][:, :], SG0[:, :], ALU.mult),
                             reads=[b_PS[ba], b_SG0], writes=[b_M1])
                        S.op("dve", OP("tensor_tensor", M2[:, :], PS[bc_][:, :], SG1[:, :], ALU.mult),
                             reads=[b_PS[bc_], b_SG1], writes=[b_M2])
                        S.op("pool", OP("tensor_tensor", MG[:, f, :], M1[:, :], M2[:, :], ALU.add),
                             reads=[b_M1, b_M2], writes=[b_MG])
                    for hf in range(2):
                        Wm, b_wm = ws.get(wi + 9 + hf)
                        for tau in range(4):
                            bk = (hf * 4 + tau) % 4
                            for kc in range(8):
                                S.op("pe", OP("matmul",
                                    PS[bk][:, :], MG[:, kc, tau * 128:(tau + 1) * 128], Wm[:, kc, :],
                                    start=(kc == 0), stop=(kc == 7)), reads=[b_wm, b_MG], writes=[b_PS[bk]],
                                     sig=(kc == 7))
                            S.op("dve", OP("tensor_tensor",
                                XT[:, tau, hf * 512:(hf + 1) * 512], PS[bk][:, :], XT[:, tau, hf * 512:(hf + 1) * 512],
                                ALU.add), reads=[b_PS[bk], b_XT], writes=[b_XT])
                    if G < 3:
                        prefetch(G + 1)
                    norm_group((XS2, SSQ, b_x2c, b_ssc), XT, b_XT, GP[:, l, :], U2, b_U2)
                    for hc in range(32):
                        if hc % 4 == 0:
                            Wu, b_wu = ws.get(wi + 11 + hc // 4)
                        bk = hc % 4
                        ho = (hc % 4) * 128
                        for kc in range(8):
                            S.op("pe", OP("matmul",
                                PS[bk][:, :], Wu[:, kc, ho:ho + 128], U2[:, kc, :], start=(kc == 0), stop=(kc == 7)),
                                 reads=[b_wu, b_U2], writes=[b_PS[bk]], sig=(kc == 7))
                        S.op("act", OP("activation", RL[hc % 2][:, :], PS[bk][:, :], AF.Relu),
                             reads=[b_PS[bk]], writes=[b_RL[hc % 2]])
                        S.op("pool", OP("tensor_tensor", HT[:, hc, :], RL[hc % 2][:, :], RL[hc % 2][:, :],
                                                                      ALU.mult),
                             reads=[b_RL[hc % 2]], writes=[b_HT])
                    for hf in range(2):
                        for hg in range(4):
                            Wd, b_wd = ws.get(wi + 19 + hf * 4 + hg)
                            for tau in range(4):
                                bk = 4 + tau
                                for k8 in range(8):
                                    hc = hg * 8 + k8
                                    S.op("pe", OP("matmul",
                                        PS[bk][:, :], HT[:, hc, tau * 128:(tau + 1) * 128], Wd[:, k8, :],
                                        start=(hc == 0), stop=(hc == 31)), reads=[b_wd, b_HT], writes=[b_PS[bk]],
                                         sig=(k8 == 7))
                        for tau in range(4):
                            bk = 4 + tau
                            S.op("dve", OP("tensor_tensor",
                                XT[:, tau, hf * 512:(hf + 1) * 512], PS[bk][:, :], XT[:, tau, hf * 512:(hf + 1) * 512],
                                ALU.add), reads=[b_PS[bk], b_XT], writes=[b_XT])
                    if not last:
                        S.dma("sp", XRd[g0:g0 + 512, :].rearrange("(t p) d -> p t d", p=128), XT[:, :, :],
                              reads=[b_XT], writes=[b_XRd], append=True)
                    else:
                        for tau in range(4):
                            S.op("act", OP("activation", XS2[tau % 2][:, :], XT[:, tau, :], AF.Square,
                                           accum_out=SSQ[:, tau:tau + 1]), reads=[b_XT],
                                 writes=[b_x2c[tau % 2], b_ssc])
                        S.op("dve", OP("tensor_scalar", SSQ[:, 4:8], SSQ[:, 0:4], 1.0 / D, EPS, ALU.mult, ALU.add),
                             reads=[b_ssc], writes=[b_ssc])
                        S.op("act", OP("activation", SSQ[:, 8:12], SSQ[:, 4:8], AF.Sqrt), reads=[b_ssc],
                             writes=[b_ssc])
                        S.op("dve", OP("reciprocal", SSQ[:, 12:16], SSQ[:, 8:12]), reads=[b_ssc], writes=[b_ssc])
                        for tau in range(4):
                            ot, bo_ = XS2[tau % 2], b_x2c[tau % 2]
                            S.op("dve", OP("scalar_tensor_tensor", ot[:, :], XT[:, tau, :], SSQ[:, 12 + tau:13 + tau],
                                           GF[:, :], ALU.mult, ALU.mult),
                                 reads=[b_XT, b_ssc, b_GF], writes=[bo_])
                            S.dma("sp", out_d[g0 + tau * 128:g0 + (tau + 1) * 128, :], ot[:, :], reads=[bo_],
                                  writes=[b_out], append=True)
                S.barrier()
                S.flush(block)
    return nc


_CACHE = {}


def _rope_tables():
    inv = (1.0 / (np.float32(500000.0) ** (np.arange(0, 16, 2, dtype=np.float32) / np.float32(16)))).astype(np.float32)
    ang = (np.arange(T, dtype=np.float32)[:, None] * inv[None, :]).astype(np.float32)
    return np.cos(ang).astype(np.float32), np.sin(ang).astype(np.float32)


def _core_inputs(inputs, debug=False):
    cos, sin = _rope_tables()
    ident = np.eye(128, dtype=np.float32)
    shared = {k: np.ascontiguousarray(np.asarray(inputs[k], dtype=np.float32)) for k in
              ("w_in", "w_attn_out", "w_conv_out", "w_mix_out", "w_mlp_up", "w_mlp_down", "norm_mix", "norm_mlp",
               "norm_final", "conv_w")}
    x = np.asarray(inputs["x"], dtype=np.float32)
    maps = []
    for core in range(NCORES):
        b, p = core // 2, core % 2
        tiles = np.arange(NT) * 2 + p
        tok = (tiles[:, None] * 128 + np.arange(128)[None, :]).reshape(-1)
        xc = np.ascontiguousarray(x[b][tok])
        c = cos[tok].T
        s = sin[tok].T
        ropeC = np.ascontiguousarray(np.concatenate([c, c], 0))
        ropeS = np.ascontiguousarray(np.concatenate([-s, s], 0))
        adm = np.zeros((128, 2, 512), np.float32)
        col = np.arange(512)[None, :]
        tt = np.arange(128)[:, None]
        for tau in range(2):
            lim = 128 * (2 * tau + p) + 64 + 64 * (tt >= 64)
            adm[:, tau, :] = np.where(col < lim, 0.0, NEG)
        sel = np.zeros((128, 2), np.float32)
        sel[:, 0] = 1.0 if p == 1 else 0.0
        sel[:, 1] = 1.0 if p == 0 else 0.0
        m = dict(shared)
        m.update({"x": xc, "ropeC": ropeC, "ropeS": ropeS, "adm": adm, "sel": sel, "ident": ident})
        maps.append(m)
    return maps


def kernel(**inputs):
    if "nc" not in _CACHE:
        _CACHE["nc"] = build_program()
    nc = _CACHE["nc"]
    maps = _core_inputs(inputs)
    res = run_bass_kernel_spmd(nc, maps, core_ids=list(range(NCORES)))
    out = np.empty((NB, T, D), np.float32)
    for core in range(NCORES):
        b, p = core // 2, core % 2
        o = np.asarray(res.results[core]["out"], dtype=np.float32).reshape(NT, 128, D)
        for j in range(NT):
            m = 2 * j + p
            out[b, m * 128:(m + 1) * 128, :] = o[j]
    return out
```
